# Optimizing a Trainium2 kernel written in Bass

```python
import jax, jax.numpy as jnp
from jax import lax
import numpy as np


D_MODEL = 1024
BATCH = 2
SEQ = 8192
DEPTH = 4

GRID_W = 64
CTX_LEN = 256
EPS = 1e-6
CONV_A_DIM = 512
CONV_A_WIDTH = 31
CONV_B_DIM = 512
CONV_B_WIDTH = 3
EA_VAL = 0
EA_GATE = EA_VAL + CONV_A_DIM
EB_B = EA_GATE + CONV_A_DIM
EB_C = EB_B + CONV_B_DIM
EB_H = EB_C + CONV_B_DIM
EVEN_IN = EB_H + CONV_B_DIM
MLA_HEADS = 8
MLA_NOPE = 64
MLA_ROPE = 32
MLA_V = 64
Q_LORA = 384
KV_LORA = 256
MLA_SCALE = (MLA_NOPE + MLA_ROPE) ** -0.5
AXIS_DIM = MLA_ROPE // 2
AXIS_PAIRS = AXIS_DIM // 2
ROPE_BASE = 10000.0
Q_BLOCK = 128
POOL_WINDOWS = (2, 4, 8, 16)
POOL_GROUP = 128
POOL_DIM = POOL_GROUP * len(POOL_WINDOWS)
KV_OFF = Q_LORA
KR_OFF = KV_OFF + KV_LORA
POOL_OFF = KR_OFF + MLA_ROPE
ODD_IN = POOL_OFF + POOL_DIM
MIX_OUT = 1024
N_GROUPS = 4
EXPERTS_PER_GROUP = 4
N_EXPERTS = N_GROUPS * EXPERTS_PER_GROUP
TOP_K = 2
D_EXPERT = 512
N_EVEN = (DEPTH + 1) // 2
N_ODD = DEPTH // 2

kernel_name = 'hybrid_conv_mla_pool_hmoe_diffusion_prefix'


def rms_norm(x, g):
    xf = x.astype(jnp.float32)
    y = xf * lax.rsqrt(jnp.mean(xf * xf, axis=-1, keepdims=True) + EPS)
    return (y * g.astype(jnp.float32)).astype(x.dtype)


def layer_norm(x, g, b):
    xf = x.astype(jnp.float32)
    xc = xf - jnp.mean(xf, axis=-1, keepdims=True)
    y = xc * lax.rsqrt(jnp.mean(xc * xc, axis=-1, keepdims=True) + EPS)
    return (y * g.astype(jnp.float32) + b.astype(jnp.float32)).astype(x.dtype)


def modulate(h, shift, scale):
    return h * (1 + scale) + shift


def depthwise_conv(x, w):
    k = w.shape[0]
    return lax.conv_general_dilated(x, w[:, None, :], window_strides=(1,), padding=[((k - 1) // 2, k // 2)],
                                    dimension_numbers=('NWC', 'WIO', 'NWC'), feature_group_count=x.shape[-1])


def even_mixer(h, w_in, conv_a_w, conv_a_b, ln_a_g, ln_a_b, conv_b_w, w_out):
    u = h @ w_in
    a = u[..., EA_VAL:EA_GATE] * jax.nn.sigmoid(u[..., EA_GATE:EB_B])
    a = depthwise_conv(a, conv_a_w) + conv_a_b
    a = jax.nn.silu(layer_norm(a, ln_a_g, ln_a_b))
    gb, gc, hb = u[..., EB_B:EB_C], u[..., EB_C:EB_H], u[..., EB_H:EVEN_IN]
    b = gb * depthwise_conv(gc * hb, conv_b_w)
    return jnp.concatenate([a, b], axis=-1) @ w_out


def apply_axial_rope(x, cos, sin):
    xf = x.astype(jnp.float32)
    out = []
    for a in range(2):
        seg = xf[..., a * AXIS_DIM:(a + 1) * AXIS_DIM]
        x1, x2 = seg[..., :AXIS_PAIRS], seg[..., AXIS_PAIRS:]
        ca = cos[..., a * AXIS_PAIRS:(a + 1) * AXIS_PAIRS]
        sa = sin[..., a * AXIS_PAIRS:(a + 1) * AXIS_PAIRS]
        out += [x1 * ca - x2 * sa, x1 * sa + x2 * ca]
    return jnp.concatenate(out, axis=-1).astype(x.dtype)


def mla_q(q_c, q_norm_g, w_uq):
    b, l, _ = q_c.shape
    q = (rms_norm(q_c, q_norm_g) @ w_uq).reshape(b, l, MLA_HEADS, MLA_NOPE + MLA_ROPE)
    return q[..., :MLA_NOPE], q[..., MLA_NOPE:]


def mla_kv(kv_c, kv_norm_g, w_ukv):
    b, l, _ = kv_c.shape
    kv = (rms_norm(kv_c, kv_norm_g) @ w_ukv).reshape(b, l, MLA_HEADS, MLA_NOPE + MLA_V)
    return kv[..., :MLA_NOPE], kv[..., MLA_NOPE:]


def mla_attend(q_nope, q_rope, k_nope, k_rope, v):
    b, lq, h, _ = q_nope.shape
    nb = lq // Q_BLOCK

    def blocks(a):
        return jnp.moveaxis(a.reshape((b, nb, Q_BLOCK) + a.shape[2:]), 1, 0)

    def one(qs):
        qn, qr = qs
        s = jnp.einsum('bqhd,bkhd->bhqk', qn, k_nope) + jnp.einsum('bqhr,bkr->bhqk', qr, k_rope)
        p = jax.nn.softmax(s.astype(jnp.float32) * MLA_SCALE, axis=-1).astype(v.dtype)
        return jnp.einsum('bhqk,bkhd->bqhd', p, v)

    out = lax.map(one, (blocks(q_nope), blocks(q_rope)))
    return jnp.moveaxis(out, 0, 1).reshape(b, lq, h * MLA_V)


def centred_mean_minus_self(x, w):
    b, l, ch = x.shape
    cs = jnp.concatenate([jnp.zeros((b, 1, ch), jnp.float32), jnp.cumsum(x.astype(jnp.float32), axis=1)], axis=1)
    t = jnp.arange(l)
    lo = jnp.clip(t - w // 2, 0, l)
    hi = jnp.clip(t - w // 2 + w, 0, l)
    mean = (cs[:, hi] - cs[:, lo]) / (hi - lo).astype(jnp.float32)[None, :, None]
    return mean.astype(x.dtype) - x


def pool_mixer(u, w_pool, b_pool, s_pool):
    b, l, _ = u.shape
    parts = [centred_mean_minus_self(u[..., g * POOL_GROUP:(g + 1) * POOL_GROUP], w) for g, w in enumerate(POOL_WINDOWS)]
    p = jnp.stack(parts, axis=2)
    y = jnp.einsum('blgc,gcd->blgd', p, w_pool) + b_pool
    return y.reshape(b, l, POOL_DIM) * s_pool


def odd_mixer(h_lat, h_ctx, cos, sin, w_in, q_norm_g, w_uq, kv_norm_g, w_ukv, w_pool, b_pool, s_pool, w_out, ctx_out):
    u = h_lat @ w_in
    o = 0 if ctx_out else KV_OFF
    u_ctx = h_ctx @ (w_in if ctx_out else w_in[:, KV_OFF:POOL_OFF])
    kn_c, v_c = mla_kv(u_ctx[..., KV_OFF - o:KR_OFF - o], kv_norm_g, w_ukv)
    kr_c = u_ctx[..., KR_OFF - o:POOL_OFF - o]
    kn_l, v_l = mla_kv(u[..., KV_OFF:KR_OFF], kv_norm_g, w_ukv)
    kr_l = apply_axial_rope(u[..., KR_OFF:POOL_OFF], cos, sin)
    qn_l, qr_l = mla_q(u[..., :KV_OFF], q_norm_g, w_uq)
    qr_l = apply_axial_rope(qr_l, cos[:, None, :], sin[:, None, :])
    att = mla_attend(qn_l, qr_l, jnp.concatenate([kn_c, kn_l], axis=1), jnp.concatenate([kr_c, kr_l], axis=1),
                     jnp.concatenate([v_c, v_l], axis=1))
    pool = pool_mixer(u[..., POOL_OFF:], w_pool, b_pool, s_pool)
    y_lat = jnp.concatenate([att, pool], axis=-1) @ w_out
    y_ctx = None
    if ctx_out:
        qn_c, qr_c = mla_q(u_ctx[..., :KV_OFF], q_norm_g, w_uq)
        att_c = mla_attend(qn_c, qr_c, kn_c, kr_c, v_c)
        pool_c = pool_mixer(u_ctx[..., POOL_OFF:], w_pool, b_pool, s_pool)
        y_ctx = jnp.concatenate([att_c, pool_c], axis=-1) @ w_out
    return y_lat, y_ctx


def hier_moe(h, wg, bg, we, be, w1, w3, w2):
    shp = h.shape
    t = h.reshape(-1, shp[-1])
    g_logits = (t @ wg + bg).astype(jnp.float32)
    g_idx = jnp.argmax(g_logits, axis=-1)
    g_w = jnp.take_along_axis(jax.nn.softmax(g_logits, axis=-1), g_idx[:, None], axis=-1)
    e_logits = (t @ we + be).astype(jnp.float32).reshape(-1, N_GROUPS, EXPERTS_PER_GROUP)
    e_in = jnp.take_along_axis(e_logits, g_idx[:, None, None], axis=1)[:, 0]
    top_v, top_i = lax.top_k(e_in, TOP_K)
    top_w = jax.nn.softmax(top_v, axis=-1) * g_w
    eid = g_idx[:, None] * EXPERTS_PER_GROUP + top_i
    gates = jnp.einsum('nk,nke->ne', top_w, jax.nn.one_hot(eid, N_EXPERTS, dtype=jnp.float32)).astype(t.dtype)
    out = jnp.zeros_like(t)
    for e in range(N_EXPERTS):
        hid = jax.nn.silu(t @ w1[e]) * (t @ w3[e])
        out = out + gates[:, e:e + 1] * (hid @ w2[e])
    return out.reshape(shp)


def setup_inputs(seed: int = 0) -> dict:
    key = jax.random.key(seed)
    ks = iter(jax.random.split(key, 40))
    D = D_MODEL

    def nrm(shape, scale):
        return jax.random.normal(next(ks), shape, jnp.float32) * scale

    def gain(shape):
        return 1.0 + nrm(shape, 0.1)

    return {
        'x': nrm((BATCH, SEQ, D), 1.0),
        'c': nrm((BATCH, D), 1.0),
        'ctx': nrm((BATCH, CTX_LEN, D), 1.0),
        'c_ctx': nrm((D,), 1.0),
        'w_mod': nrm((DEPTH, D, 6 * D), 0.5 * D ** -0.5),
        'b_mod': nrm((DEPTH, 6 * D), 0.02),
        'norm_g': gain((DEPTH, 2, D)),
        'ev_w_in': nrm((N_EVEN, D, EVEN_IN), D ** -0.5),
        'ev_conv_a_w': nrm((N_EVEN, CONV_A_WIDTH, CONV_A_DIM), CONV_A_WIDTH ** -0.5),
        'ev_conv_a_b': nrm((N_EVEN, CONV_A_DIM), 0.02),
        'ev_ln_a_g': gain((N_EVEN, CONV_A_DIM)),
        'ev_ln_a_b': nrm((N_EVEN, CONV_A_DIM), 0.02),
        'ev_conv_b_w': nrm((N_EVEN, CONV_B_WIDTH, CONV_B_DIM), CONV_B_WIDTH ** -0.5),
        'ev_w_out': nrm((N_EVEN, MIX_OUT, D), MIX_OUT ** -0.5),
        'od_w_in': nrm((N_ODD, D, ODD_IN), D ** -0.5),
        'od_q_norm_g': gain((N_ODD, Q_LORA)),
        'od_w_uq': nrm((N_ODD, Q_LORA, MLA_HEADS * (MLA_NOPE + MLA_ROPE)), Q_LORA ** -0.5),
        'od_kv_norm_g': gain((N_ODD, KV_LORA)),
        'od_w_ukv': nrm((N_ODD, KV_LORA, MLA_HEADS * (MLA_NOPE + MLA_V)), KV_LORA ** -0.5),
        'od_w_pool': nrm((N_ODD, len(POOL_WINDOWS), POOL_GROUP, POOL_GROUP), POOL_GROUP ** -0.5),
        'od_b_pool': nrm((N_ODD, len(POOL_WINDOWS), POOL_GROUP), 0.02),
        'od_s_pool': gain((N_ODD, POOL_DIM)),
        'od_w_out': nrm((N_ODD, MIX_OUT, D), MIX_OUT ** -0.5),
        'moe_wg': nrm((DEPTH, D, N_GROUPS), D ** -0.5),
        'moe_bg': nrm((DEPTH, N_GROUPS), 0.01),
        'moe_we': nrm((DEPTH, D, N_EXPERTS), D ** -0.5),
        'moe_be': nrm((DEPTH, N_EXPERTS), 0.01),
        'moe_w1': nrm((DEPTH, N_EXPERTS, D, D_EXPERT), D ** -0.5),
        'moe_w3': nrm((DEPTH, N_EXPERTS, D, D_EXPERT), D ** -0.5),
        'moe_w2': nrm((DEPTH, N_EXPERTS, D_EXPERT, D), D_EXPERT ** -0.5),
        'final_g': gain((D,)),
    }


def reference(x, c, ctx, c_ctx, w_mod, b_mod, norm_g, ev_w_in, ev_conv_a_w, ev_conv_a_b, ev_ln_a_g, ev_ln_a_b,
              ev_conv_b_w, ev_w_out, od_w_in, od_q_norm_g, od_w_uq, od_kv_norm_g, od_w_ukv, od_w_pool, od_b_pool,
              od_s_pool, od_w_out, moe_wg, moe_bg, moe_we, moe_be, moe_w1, moe_w3, moe_w2, final_g):
    n_tok = x.shape[1]
    rows = n_tok // GRID_W
    pos_row = jnp.repeat(jnp.arange(rows, dtype=jnp.float32), GRID_W)
    pos_col = jnp.tile(jnp.arange(GRID_W, dtype=jnp.float32), rows)
    inv = ROPE_BASE ** (-jnp.arange(0, AXIS_DIM, 2, dtype=jnp.float32) / AXIS_DIM)
    ang = jnp.concatenate([pos_row[:, None] * inv, pos_col[:, None] * inv], axis=-1)
    cos, sin = jnp.cos(ang), jnp.sin(ang)
    s_lat = jax.nn.silu(c)[:, None, :]
    s_ctx = jax.nn.silu(c_ctx)[None, None, :]
    xc = ctx
    for i in range(DEPTH):
        j = i // 2
        last = i == DEPTH - 1
        odd = i % 2 == 1
        m_lat = jnp.split(s_lat @ w_mod[i] + b_mod[i], 6, axis=-1)
        hl = modulate(rms_norm(x, norm_g[i, 0]), m_lat[0], m_lat[1])
        if odd or not last:
            m_ctx = jnp.split(s_ctx @ w_mod[i] + b_mod[i], 6, axis=-1)
            hc = modulate(rms_norm(xc, norm_g[i, 0]), m_ctx[0], m_ctx[1])
        if odd:
            yl, yc = odd_mixer(hl, hc, cos, sin, od_w_in[j], od_q_norm_g[j], od_w_uq[j], od_kv_norm_g[j], od_w_ukv[j],
                               od_w_pool[j], od_b_pool[j], od_s_pool[j], od_w_out[j], not last)
        else:
            ev = (ev_w_in[j], ev_conv_a_w[j], ev_conv_a_b[j], ev_ln_a_g[j], ev_ln_a_b[j], ev_conv_b_w[j], ev_w_out[j])
            yl = even_mixer(hl, *ev)
            yc = None if last else even_mixer(hc, *ev)
        moe = (moe_wg[i], moe_bg[i], moe_we[i], moe_be[i], moe_w1[i], moe_w3[i], moe_w2[i])
        x = x + m_lat[2] * yl
        x = x + m_lat[5] * hier_moe(modulate(rms_norm(x, norm_g[i, 1]), m_lat[3], m_lat[4]), *moe)
        if not last:
            xc = xc + m_ctx[2] * yc
            xc = xc + m_ctx[5] * hier_moe(modulate(rms_norm(xc, norm_g[i, 1]), m_ctx[3], m_ctx[4]), *moe)
    return rms_norm(x, final_g)
```

```python
import numpy as np
import concourse.bass as bass
import concourse.mybir as mybir
from concourse.bass_utils import run_bass_kernel_spmd

F32 = mybir.dt.float32
BF16 = mybir.dt.bfloat16
ALU = mybir.AluOpType
AF = mybir.ActivationFunctionType

D = 1024
DC = 8
HALO = 64
NOWN = 2048
NLAT = NOWN + 2 * HALO
NCTX = 256
NT = NLAT + NCTX
PADW = 16
NPAD = PADW + NLAT + PADW + NCTX + PADW
EPS = 1e-6
SEQ = 8192
NKEY = NCTX + SEQ
NKT = NKEY // 128
MLA_SCALE = 96 ** -0.5
BLOCKS = [(0, 512), (512, 1024), (1024, 1536), (1536, 2048), (2048, 2432)]


def pcol(c):
    return c + PADW if c < NLAT else c + 2 * PADW


def segs(c0, c1):
    out = []
    if c0 < NLAT:
        out.append((c0, min(c1, NLAT), 0))
    if c1 > NLAT:
        out.append((max(c0, NLAT), c1, 1))
    return out


class _Rec:
    def __init__(self):
        self.call = None

    def __getattr__(self, name):
        def f(*a, **k):
            self.call = (name, a, k)
            return self
        return f


def _eager(fn):
    rec = _Rec()
    fn(rec)
    name, a, k = rec.call
    return lambda e: getattr(e, name)(*a, **k)


class Prog:
    CE = ("pe", "act", "dve", "pool")

    def __init__(self, nc, n_dma_sems=10):
        self.nc = nc
        self.streams = {e: [] for e in ("pe", "act", "dve", "pool", "sp")}
        self.sems = {}
        self.semval = {}
        for e in self.CE:
            self.sems[e] = nc.alloc_semaphore("s_" + e)
            self.semval[e] = 0
        self.dma_sems = {}
        for q in ("sp", "pool"):
            self.dma_sems[q] = []
            for i in range(n_dma_sems):
                k = "d_%s%d" % (q, i)
                self.sems[k] = nc.alloc_semaphore(k)
                self.semval[k] = 0
                self.dma_sems[q].append(k)
        self.dma_rr = {"sp": 0, "pool": 0}
        self.known = {e: {} for e in self.streams}
        self.last_w = {}
        self.readers = {}
        self.nops = 0

    def _deps(self, eng, r, w):
        deps = {}
        def add(ev, raw):
            if ev is None:
                return
            k, v = ev
            if k == eng:
                if not raw or eng == "pe" or v < self.semval[eng] - 1:
                    return
            if deps.get(k, 0) < v:
                deps[k] = v
        for x in r:
            add(self.last_w.get(x), True)
        for x in w:
            add(self.last_w.get(x), False)
            for ev in self.readers.get(x, ()):
                add(ev, False)
        return deps

    def _emit_waits(self, eng, deps):
        kn = self.known[eng]
        for k, v in deps.items():
            if kn.get(k, 0) >= v:
                continue
            kn[k] = v
            sem = self.sems[k]
            self.streams[eng].append(("wait", sem, v))

    def _commit(self, ev, r, w):
        for x in r:
            self.readers.setdefault(x, []).append(ev)
        for x in w:
            self.last_w[x] = ev
            self.readers[x] = []

    def op(self, eng, fn, r=(), w=()):
        deps = self._deps(eng, r, w)
        self._emit_waits(eng, deps)
        self.semval[eng] += 1
        ev = (eng, self.semval[eng])
        self.streams[eng].append(("op", _eager(fn), self.sems[eng], 1))
        self._commit(ev, r, w)
        self.nops += 1
        return ev

    def dma(self, q, out, in_, r=(), w=()):
        sems = self.dma_sems[q]
        k = sems[self.dma_rr[q] % len(sems)]
        self.dma_rr[q] += 1
        deps = self._deps(q, r, w)
        deps[k] = max(deps.get(k, 0), self.semval[k])
        self._emit_waits(q, deps)
        self.semval[k] += 16
        ev = (k, self.semval[k])
        self.streams[q].append(("op", lambda e, o=out, i=in_: e.dma_start(out=o, in_=i), self.sems[k], 16))
        self._commit(ev, r, w)
        self.nops += 1
        return ev

    def custom(self, q, fn, semkey_inc, r=(), w=()):
        k, inc = semkey_inc
        deps = self._deps(q, r, w)
        deps[k] = max(deps.get(k, 0), self.semval[k])
        self._emit_waits(q, deps)
        self.semval[k] += (inc if inc else 1)
        ev = (k, self.semval[k])
        self.streams[q].append(("op", _eager(fn), self.sems[k], inc))
        self._commit(ev, r, w)
        return ev

    def wait_all(self, q, res):
        deps = self._deps(q, res, res)
        self._emit_waits(q, deps)

    def run(self):
        nc = self.nc
        engs = {"pe": "tensor", "act": "scalar", "dve": "vector", "pool": "gpsimd", "sp": "sync"}
        with nc.Block() as block:
            for name, attr in engs.items():
                stream = self.streams[name]
                def body(e, stream=stream):
                    for it in stream:
                        if it[0] == "wait":
                            e.wait_ge(it[1], it[2])
                        else:
                            ins = it[1](e)
                            if it[3]:
                                ins.then_inc(it[2], it[3])
                            else:
                                ins.then_inc(it[2])
                getattr(block, attr)(body)


def _pf_map():
    m = {}
    off = 0
    def add(name, n):
        nonlocal off
        m[name] = (off, n)
        off += n
    add("ident", 128)
    add("cvec", 16)
    add("b_mod", 4 * 48)
    add("norm_g", 4 * 2 * 8)
    add("final_g", 8)
    add("conv_a_w", 2 * 4 * 31)
    add("conv_a_b", 8)
    add("ln_g", 8)
    add("ln_b", 8)
    add("conv_b_w", 2 * 4 * 3)
    add("q_g", 6)
    add("kv_g", 4)
    add("b_pool", 8)
    add("s_pool", 8)
    add("moe_b", 4 * 20)
    add("mask", 128)
    add("pcorr_lat", 4 * 32)
    add("pcorr_ctx", 4 * 32)
    m["_n"] = off
    return m


PFM = _pf_map()
NPF = ((PFM["_n"] + 63) // 64) * 64


def fm(v):
    v = np.asarray(v, np.float32)
    lead = v.shape[:-1]
    n = v.shape[-1] // 128
    v = v.reshape(lead + (n, 128))
    v = np.moveaxis(v, -1, 0)
    return np.ascontiguousarray(v).reshape(128, -1)


def host_tables(core):
    b, r = core // 4, core % 4
    pos = r * NOWN - HALO + np.arange(NLAT)
    valid = (pos >= 0) & (pos < SEQ)
    posc = np.clip(pos, 0, SEQ - 1)
    inv = (10000.0 ** (-np.arange(0, 16, 2, dtype=np.float32) / 16)).astype(np.float32)
    ang = np.concatenate([(posc // 64).astype(np.float32)[:, None] * inv,
                          (posc % 64).astype(np.float32)[:, None] * inv], axis=-1)
    cos, sin = np.cos(ang).astype(np.float32), np.sin(ang).astype(np.float32)
    C = np.zeros((32, NLAT), np.float32)
    S = np.zeros((32, NLAT), np.float32)
    for a in range(2):
        ca, sa = cos[:, 8 * a:8 * a + 8].T, sin[:, 8 * a:8 * a + 8].T
        C[16 * a:16 * a + 8] = ca
        C[16 * a + 8:16 * a + 16] = ca
        S[16 * a:16 * a + 8] = -sa
        S[16 * a + 8:16 * a + 16] = sa
    rope = np.zeros((128, 2, NLAT), np.float32)
    rope[64:96, 0] = C
    rope[64:96, 1] = S
    mask = np.concatenate([valid[:HALO], valid[NLAT - HALO:]]).astype(np.float32)
    mask = np.tile(mask[None, :], (128, 1))

    def corr(posarr, L):
        out = np.ones((4, len(posarr)), np.float32)
        for g, w in enumerate((2, 4, 8, 16)):
            lo = np.clip(posarr - w // 2, 0, L)
            hi = np.clip(posarr - w // 2 + w, 0, L)
            cnt = np.maximum(hi - lo, 1)
            out[g] = w / cnt.astype(np.float32)
        return out
    le = np.concatenate([np.arange(HALO - 8, HALO + 8), np.arange(NLAT - HALO - 8, NLAT - HALO + 8)])
    pc_lat = corr(pos[le], SEQ)
    pc_lat[:, ~valid[le]] = 1.0
    ce = np.concatenate([np.arange(0, 16), np.arange(NCTX - 16, NCTX)])
    pc_ctx = corr(ce, NCTX)
    return rope, mask, np.tile(pc_lat.reshape(1, -1), (128, 1)), np.tile(pc_ctx.reshape(1, -1), (128, 1))


def host_pf(inp, core):
    b = core // 4
    pf = np.zeros((128, NPF), np.float32)
    def put(name, arr):
        o, n = PFM[name]
        arr = np.asarray(arr, np.float32).reshape(128, -1)
        assert arr.shape[1] == n, (name, arr.shape, n)
        pf[:, o:o + n] = arr
    put("ident", np.eye(128, dtype=np.float32))
    cv = np.stack([fm(inp["c"][b]), fm(inp["c_ctx"])], axis=-1)
    put("cvec", cv)
    put("b_mod", fm(inp["b_mod"].reshape(4, 6, 1024)))
    put("norm_g", fm(inp["norm_g"]))
    put("final_g", fm(inp["final_g"]))
    caw = np.transpose(inp["ev_conv_a_w"].reshape(2, 31, 4, 128), (3, 0, 2, 1))
    put("conv_a_w", caw)
    put("conv_a_b", fm(inp["ev_conv_a_b"]))
    put("ln_g", fm(inp["ev_ln_a_g"]))
    put("ln_b", fm(inp["ev_ln_a_b"]))
    put("conv_b_w", np.transpose(inp["ev_conv_b_w"].reshape(2, 3, 4, 128), (3, 0, 2, 1)))
    put("q_g", fm(inp["od_q_norm_g"]))
    put("kv_g", fm(inp["od_kv_norm_g"]))
    put("b_pool", fm(inp["od_b_pool"].reshape(2, 512)))
    put("s_pool", fm(inp["od_s_pool"]))
    mb = np.concatenate([inp["moe_bg"], inp["moe_be"]], axis=-1).reshape(1, 80)
    put("moe_b", np.tile(mb, (128, 1)))
    rope, mask, pcl, pcc = host_tables(core)
    put("mask", mask)
    put("pcorr_lat", pcl)
    put("pcorr_ctx", pcc)
    return pf, rope


def host_weights(inp):
    w = {}
    f = lambda a: np.ascontiguousarray(np.asarray(a, np.float32))
    w["w_mod"] = f(inp["w_mod"])
    w["ev_w_in"] = f(inp["ev_w_in"])
    w["ev_w_out"] = f(inp["ev_w_out"])
    wi = np.asarray(inp["od_w_in"], np.float32)
    q, kv, kr, pool = wi[..., :384], wi[..., 384:640], wi[..., 640:672], wi[..., 672:]
    swap = np.concatenate([np.arange(8, 16), np.arange(0, 8), np.arange(24, 32), np.arange(16, 24)])
    z64 = np.zeros(kr.shape[:-1] + (64,), np.float32)
    w["od_w_in"] = f(np.concatenate([q, kv, pool, z64, kr, z64, kr[..., swap]], axis=-1))
    uq = np.asarray(inp["od_w_uq"], np.float32).reshape(2, 384, 8, 96)
    uqs = np.zeros_like(uq)
    uqs[..., 64:] = uq[..., 64:][..., swap]
    w["od_w_uq"] = f(uq.reshape(2, 384, 768))
    w["od_w_uqs"] = f(uqs.reshape(2, 384, 768))
    w["od_w_ukv"] = f(inp["od_w_ukv"])
    w["od_w_pool"] = f(inp["od_w_pool"])
    w["od_w_out"] = f(inp["od_w_out"])
    w["moe_wge"] = f(np.concatenate([inp["moe_wg"], inp["moe_we"]], axis=-1))
    w["moe_w1"] = f(inp["moe_w1"])
    w["moe_w3"] = f(inp["moe_w3"])
    w["moe_w2"] = f(inp["moe_w2"])
    return w


AW = 52800
OFF_PF = 0
OFF_MOD = OFF_PF + NPF
OFF_ONES = OFF_MOD + 384
OFF_SEL = OFF_ONES + 64
OFF_SELND = OFF_SEL + 1024
OFF_XT = OFF_SELND + 512
OFF_S = OFF_XT + DC * NT
SW = AW - OFF_S


class Builder:
    def __init__(self, layers, in_mode, out_mode):
        self.layers = list(layers)
        self.in_mode = in_mode
        self.out_mode = out_mode
        nc = bass.Bass("TRN2", target_bir_lowering=False)
        self.nc = nc
        self.P = Prog(nc)
        self.arena = nc.alloc_sbuf_tensor("arena", [128, AW], F32).ap()
        self.PS = nc.alloc_psum_tensor("ps", [128, 8, 512], F32).ap()
        self.dram = {}
        self._bank = 0

    def din(self, name, shape, dt=F32):
        if name not in self.dram:
            self.dram[name] = self.nc.dram_tensor(name, list(shape), dt, kind="ExternalInput").ap()
        return self.dram[name]

    def dout(self, name, shape, dt=F32):
        self.dram[name] = self.nc.dram_tensor(name, list(shape), dt, kind="ExternalOutput").ap()
        return self.dram[name]

    def carve(self, off, shape, dt=F32):
        n = int(np.prod(shape[1:]))
        words = n if dt == F32 else (n + 1) // 2
        ap = self.arena[0:shape[0], off:off + words]
        if dt != F32:
            ap = ap.bitcast(dt)
        if len(shape) == 3:
            ap = ap.rearrange("p (a b) -> p a b", a=shape[1])
        elif len(shape) == 4:
            ap = ap.rearrange("p (a b c) -> p a b c", a=shape[1], b=shape[2])
        return ap, off + words

    def pf(self, name):
        o, n = PFM[name]
        return self.arena[:, OFF_PF + o:OFF_PF + o + n]

    def barrier(self):
        P = self.P
        for q in P.streams:
            for k, v in P.semval.items():
                if k == q or v == 0:
                    continue
                if P.known[q].get(k, 0) < v:
                    P.known[q][k] = v
                    P.streams[q].append(("wait", P.sems[k], v))

    def mod(self, l, j, s, c):
        o = OFF_MOD + ((l * 6 + j) * 8 + c) * 2 + s
        return self.arena[:, o:o + 1]

    def prologue(self):
        P, A = self.P, self.arena
        pf_d = self.din("pf", [128, NPF])
        P.dma("sp", A[:, OFF_PF:OFF_PF + NPF], pf_d, w=["PF"])
        self.ONES, _ = self.carve(OFF_ONES, [128, 128], BF16)
        P.op("pool", lambda e: e.memset(self.ONES, 1.0), w=["ONES"])
        self.ID = self.pf("ident")
        self.SEL, _ = self.carve(OFF_SEL, [16, 16, 128], BF16)
        for ex in range(16):
            P.op("dve", lambda e, ex=ex: e.tensor_scalar(out=self.SEL[:, ex, :], in0=self.ONES[0:16, :],
                                                         scalar1=self.ID[0:16, ex:ex + 1], scalar2=None, op0=ALU.mult),
                 r=["PF", "ONES"], w=["SEL"])
        self.SELND, _ = self.carve(OFF_SELND, [65, 4, 128], F32)
        P.op("pool", lambda e: e.memset(self.SELND, 0.0), w=["SELND"])
        P.op("dve", lambda e: e.tensor_copy(out=self.SELND[0:64, 0, 0:64], in_=self.ID[0:64, 0:64]), r=["PF", "SELND"], w=["SELND"])
        P.op("dve", lambda e: e.tensor_copy(out=self.SELND[0:64, 2, 64:128], in_=self.ID[0:64, 0:64]), r=["PF", "SELND"], w=["SELND"])
        P.op("dve", lambda e: e.memset(self.SELND[64:65, 1, 0:64], 1.0), r=["SELND"], w=["SELND"])
        P.op("dve", lambda e: e.memset(self.SELND[64:65, 3, 64:128], 1.0), r=["SELND"], w=["SELND"])
        self.XT, _ = self.carve(OFF_XT, [128, DC, NT], F32)

    def modulation(self):
        P, A, PS = self.P, self.arena, self.PS
        nl = len(self.layers)
        wm = self.din("w_mod", [nl, 1024, 6144])
        off = OFF_S + 2048
        ST, off = self.carve(off, [128, 8, 2], BF16)
        WM = []
        NWM = 6
        for i in range(NWM):
            t, off = self.carve(off, [128, 8, 1024], BF16)
            WM.append(t)
        assert off <= AW
        P.op("act", lambda e: e.activation(out=ST.rearrange("p a b -> p (a b)"), in_=self.pf("cvec"), func=AF.Silu), r=["PF"], w=["ST"])
        n = 0
        for li, l in enumerate(self.layers):
            bank = 4 + li % 2
            for j in range(6):
                slot = n % NWM
                n += 1
                P.dma("pool", WM[slot], wm[li, :, j * 1024:(j + 1) * 1024].rearrange("(c p) m -> p c m", p=128), w=[("WM", slot)])
                for mc in range(8):
                    col = (j * 8 + mc) * 2
                    for k in range(8):
                        P.op("pe", lambda e, slot=slot, mc=mc, k=k, col=col, bank=bank: e.matmul(
                            PS[:, bank, col:col + 2], lhsT=WM[slot][:, k, mc * 128:(mc + 1) * 128], rhs=ST[:, k, :],
                            start=(k == 0), stop=(k == 7)), r=[("WM", slot), "ST"], w=[("PS", bank)])
            o, _ = PFM["b_mod"]
            BM = A[:, OFF_PF + o + l * 48:OFF_PF + o + (l + 1) * 48]
            MODL = A[:, OFF_MOD + li * 96:OFF_MOD + (li + 1) * 96].rearrange("p (a s) -> p a s", s=2)
            PSV = PS[:, bank, 0:96].rearrange("p (a s) -> p a s", s=2)
            for s in range(2):
                P.op("dve", lambda e, s=s, MODL=MODL, PSV=PSV, BM=BM: e.tensor_tensor(out=MODL[:, :, s], in0=PSV[:, :, s], in1=BM, op=ALU.add),
                     r=[("PS", bank), "PF"], w=["MOD"])
            o, _ = PFM["norm_g"]
            for (j, gi) in ((1, 0), (4, 1)):
                G = A[:, OFF_PF + o + (l * 2 + gi) * 8:OFF_PF + o + (l * 2 + gi) * 8 + 8]
                for s in range(2):
                    V = MODL[:, j * 8:(j + 1) * 8, s]
                    P.op("dve", lambda e, V=V, G=G: e.scalar_tensor_tensor(out=V, in0=V, scalar=1.0, in1=G, op0=ALU.add, op1=ALU.mult),
                         r=["MOD", "PF"], w=["MOD"])
        self.barrier()

    def load_x_raw(self):
        P, PS, XT = self.P, self.PS, self.XT
        x_in = self.din("x_in", [NLAT, 1024])
        c_in = self.din("ctx_in", [NCTX, 1024])
        off = OFF_S
        XS = []
        for i in range(2):
            t, off = self.carve(off, [128, 1024], F32)
            XS.append(t)
        for t in range(NT // 128):
            slot = t % 2
            src = x_in[t * 128:(t + 1) * 128, :] if t < NLAT // 128 else c_in[(t - NLAT // 128) * 128:(t - NLAT // 128 + 1) * 128, :]
            P.dma("sp", XS[slot], src, w=[("XS", slot)])
            for half in range(2):
                bank = 2 * slot + half
                for c4 in range(4):
                    c = half * 4 + c4
                    P.op("pe", lambda e, slot=slot, c=c, c4=c4, bank=bank: e.transpose(
                        out=PS[:, bank, c4 * 128:(c4 + 1) * 128], in_=XS[slot][:, c * 128:(c + 1) * 128], identity=self.ID),
                        r=[("XS", slot), "PF"], w=[("PS", bank)])
                dst = XT[:, half * 4:half * 4 + 4, t * 128:(t + 1) * 128]
                srcp = PS[:, bank, :].rearrange("p (a b) -> p a b", a=4)
                if half == 0:
                    P.op("act", lambda e, dst=dst, srcp=srcp: e.activation(out=dst, in_=srcp, func=AF.Copy), r=[("PS", bank)], w=[("XT", t // 4 if t < 16 else 4)])
                else:
                    P.op("dve", lambda e, dst=dst, srcp=srcp: e.tensor_copy(out=dst, in_=srcp), r=[("PS", bank)], w=[("XT", t // 4 if t < 16 else 4)])

    def load_x_state(self):
        st = self.din("st_in", [128, DC * NT])
        for b, (c0, c1) in enumerate(BLOCKS):
            self.P.dma("sp", self.XT[:, :, c0:c1], st.rearrange("p (a b) -> p a b", a=DC)[:, :, c0:c1], w=[("XT", b)])
        self.barrier()

    def store_x_state(self):
        self.barrier()
        st = self.dout("st_out", [128, DC * NT])
        for b, (c0, c1) in enumerate(BLOCKS):
            self.P.dma("sp", st.rearrange("p (a b) -> p a b", a=DC)[:, :, c0:c1], self.XT[:, :, c0:c1], r=[("XT", b)], w=["OUT"])
        self.P.wait_all("sp", ["OUT"])

    def norm_scratch(self, off):
        self.nSQ, off = self.carve(off, [128, DC, 512], BF16)
        self.nRS, off = self.carve(off, [128, 512], F32)
        self.nTM = []
        for i in range(2):
            t, off = self.carve(off, [128, 512], F32)
            self.nTM.append(t)
        self._ntm = 0
        return off

    def norm_mod(self, b, dst_fn, lslot, jA, jB, wres, rng=None, psum_rs=False):
        P, PS, XT = self.P, self.PS, self.XT
        c0, c1 = rng if rng is not None else BLOCKS[b]
        nb = c1 - c0
        SQ = self.nSQ
        if psum_rs:
            self._nrs = getattr(self, "_nrs", 0) + 1
            ri = self._nrs % 2
            RS = PS[:, ri, :]
            rsr = ("PS", ri)
        else:
            RS = self.nRS
            rsr = "nRS"
        P.op("act", lambda e: e.activation(out=SQ[:, :, :nb], in_=XT[:, :, c0:c1], func=AF.Square), r=[("XT", b)], w=["nSQ"])
        for c in range(DC):
            P.op("pe", lambda e, c=c: e.matmul(PS[:, 7, :nb], lhsT=self.ONES, rhs=SQ[:, c, :nb], start=(c == 0), stop=(c == DC - 1)),
                 r=["nSQ", "ONES"], w=[("PS", 7)])
        P.op("dve", lambda e: e.tensor_scalar(out=RS[:, :nb], in0=PS[:, 7, :nb], scalar1=1.0 / D, scalar2=EPS, op0=ALU.mult, op1=ALU.add),
             r=[("PS", 7)], w=[rsr])
        P.op("act", lambda e: e.activation(out=RS[:, :nb], in_=RS[:, :nb], func=AF.Sqrt), r=[rsr], w=[rsr])
        P.op("dve", lambda e: e.reciprocal(out=RS[:, :nb], in_=RS[:, :nb]), r=[rsr], w=[rsr])
        for c in range(DC):
            for (s0, s1, s) in segs(c0, c1):
                i = self._ntm % 2
                self._ntm += 1
                TM = self.nTM[i]
                n = s1 - s0
                P.op("dve", lambda e, c=c, s0=s0, s1=s1, s=s, TM=TM, n=n: e.scalar_tensor_tensor(
                    out=TM[:, :n], in0=XT[:, c, s0:s1], scalar=self.mod(lslot, jA, s, c), in1=RS[:, s0 - c0:s1 - c0],
                    op0=ALU.mult, op1=ALU.mult), r=[("XT", b), rsr, "MOD"], w=[("nTM", i)])
                dst = dst_fn(c, s0, s1)
                P.op("act", lambda e, dst=dst, TM=TM, n=n, c=c, s=s: e.activation(
                    out=dst, in_=TM[:, :n], func=AF.Identity, bias=self.mod(lslot, jB, s, c), scale=1.0),
                    r=[("nTM", i), "MOD"], w=wres)

    def even_mixer(self, lslot, l):
        P, PS, XT, A = self.P, self.PS, self.XT, self.arena
        j = l // 2
        nev = len([x for x in self.layers if x % 2 == 0])
        jslot = [x for x in self.layers if x % 2 == 0].index(l)
        w_in = self.din("ev_w_in", [nev, 1024, 2560])
        w_out = self.din("ev_w_out", [nev, 1024, 1024])
        off = OFF_S
        HL, off = self.carve(off, [128, DC, NT], BF16)
        APAD, off = self.carve(off, [128, 4, NPAD], BF16)
        GCH, off = self.carve(off, [128, 4, NPAD], BF16)
        GB, off = self.carve(off, [128, 4, NT], BF16)
        offW = off
        WP = []
        for i in range(2):
            t, off = self.carve(off, [128, DC, 512], BF16)
            WP.append(t)
        SG = []
        for i in range(2):
            t, off = self.carve(off, [128, 512], F32)
            SG.append(t)
        assert off <= AW, off
        self.norm_scratch(OFF_S + DC * NT // 2 + 4 * NPAD // 2)
        P.op("pool", lambda e: e.memset(APAD, 0.0), w=["APAD"])
        P.dma("pool", WP[0], w_in[jslot, :, 0:512].rearrange("(c p) m -> p c m", p=128), w=[("WP", 0)])
        P.dma("pool", WP[1], w_in[jslot, :, 512:1024].rearrange("(c p) m -> p c m", p=128), w=[("WP", 1)])
        for b in range(5):
            self.norm_mod(b, lambda c, s0, s1: HL[:, c, s0:s1], lslot, 1, 0, [("HL", b)], psum_rs=True)
        self.barrier()
        P.op("pool", lambda e: e.memset(GCH, 0.0), w=["GCH"])
        MASK = self.pf("mask")
        def load_piece(slot, pc):
            P.dma("pool", WP[slot], w_in[jslot, :, pc * 512:(pc + 1) * 512].rearrange("(c p) m -> p c m", p=128), w=[("WP", slot)])
        def mm_piece(slot, b, mc, bank):
            c0, c1 = BLOCKS[b]
            for k in range(DC):
                P.op("pe", lambda e, k=k: e.matmul(PS[:, bank, :c1 - c0], lhsT=WP[slot][:, k, mc * 128:(mc + 1) * 128], rhs=HL[:, k, c0:c1],
                                                   start=(k == 0), stop=(k == DC - 1)), r=[("WP", slot), ("HL", b)], w=[("PS", bank)])
        for (p0, p1, DST, dres) in ((0, 1, APAD, "APAD"), (3, 4, GCH, "GCH")):
            if p0 != 0:
                load_piece(0, p0)
                load_piece(1, p1)
            n = 0
            for b in range(5):
                c0, c1 = BLOCKS[b]
                nb = c1 - c0
                for mc in range(4):
                    b0, b1 = (n % 2) * 2, (n % 2) * 2 + 1
                    sg = SG[n % 2]
                    n += 1
                    mm_piece(0, b, mc, b0)
                    mm_piece(1, b, mc, b1)
                    if DST is APAD:
                        P.op("act", lambda e, sg=sg, b1=b1, nb=nb: e.activation(out=sg[:, :nb], in_=PS[:, b1, :nb], func=AF.Sigmoid),
                             r=[("PS", b1)], w=[("SG", n % 2)])
                    else:
                        P.op("act", lambda e, sg=sg, b1=b1, nb=nb: e.activation(out=sg[:, :nb], in_=PS[:, b1, :nb], func=AF.Copy),
                             r=[("PS", b1)], w=[("SG", n % 2)])
                    for (s0, s1, s) in segs(c0, c1):
                        P.op("dve", lambda e, sg=sg, b0=b0, s0=s0, s1=s1, mc=mc, DST=DST: e.tensor_tensor(
                            out=DST[:, mc, pcol(s0):pcol(s0) + s1 - s0], in0=PS[:, b0, s0 - c0:s1 - c0], in1=sg[:, s0 - c0:s1 - c0], op=ALU.mult),
                            r=[("PS", b0), ("SG", n % 2)], w=[dres])
            for mc in range(4):
                for (pc0, m0) in ((PADW, 0), (PADW + NLAT - HALO, 64)):
                    P.op("pool", lambda e, mc=mc, pc0=pc0, m0=m0, DST=DST: e.tensor_tensor(
                        out=DST[:, mc, pc0:pc0 + HALO], in0=DST[:, mc, pc0:pc0 + HALO], in1=MASK[:, m0:m0 + HALO], op=ALU.mult),
                        r=[dres, "PF"], w=[dres])
        load_piece(0, 2)
        n = 0
        for b in range(5):
            c0, c1 = BLOCKS[b]
            nb = c1 - c0
            for mc in range(4):
                bank = n % 4
                n += 1
                mm_piece(0, b, mc, bank)
                P.op("act", lambda e, bank=bank, mc=mc, c0=c0, c1=c1, nb=nb: e.activation(out=GB[:, mc, c0:c1], in_=PS[:, bank, :nb], func=AF.Copy),
                     r=[("PS", bank)], w=["GB"])
        self.barrier()
        off = OFF_S
        AOUT, off = self.carve(off, [128, 4, NT], BF16)
        DIAG, off = self.carve(off, [128, 31, 128], BF16)
        IDB, off = self.carve(off, [128, 128], BF16)
        SQ2, off = self.carve(off, [128, 4, 512], BF16)
        MEAN, off = self.carve(off, [128, 512], F32)
        VAR, off = self.carve(off, [128, 512], F32)
        TA, off = self.carve(off, [128, 512], F32)
        assert off <= OFF_S + DC * NT // 2, (off, OFF_S + DC * NT // 2)
        off = offW
        WO, off = self.carve(off, [128, DC, 1024], BF16)
        C3a, off = self.carve(off, [128, 512], F32)
        C3b, off = self.carve(off, [128, 512], F32)
        assert off <= AW
        P.dma("pool", WO, w_out[jslot].rearrange("(c p) m -> p c m", p=128), w=["WO"])
        P.op("dve", lambda e: e.tensor_copy(out=IDB, in_=self.ID), r=["PF"], w=["IDB"])
        o, _ = PFM["conv_a_w"]
        CAW = A[:, OFF_PF + o + j * 124:OFF_PF + o + (j + 1) * 124].rearrange("p (a b) -> p a b", a=4)
        o, _ = PFM["conv_a_b"]
        CAB = A[:, OFF_PF + o + j * 4:OFF_PF + o + (j + 1) * 4]
        o, _ = PFM["ln_g"]
        LG = A[:, OFF_PF + o + j * 4:OFF_PF + o + (j + 1) * 4]
        o, _ = PFM["ln_b"]
        LB = A[:, OFF_PF + o + j * 4:OFF_PF + o + (j + 1) * 4]
        o, _ = PFM["conv_b_w"]
        CBW = A[:, OFF_PF + o + j * 12:OFF_PF + o + (j + 1) * 12].rearrange("p (a b) -> p a b", a=4)
        cblocks = [(PADW + i * 512, 512, i * 512) for i in range(4)] + [(PADW + 2048, 128, 2048), (pcol(NLAT), NCTX, NLAT)]
        nb_ = 0
        for mc in range(4):
            for k in range(31):
                P.op("act", lambda e, k=k: e.activation(out=DIAG[:, k, :], in_=IDB, func=AF.Copy, scale=CAW[:, mc, k:k + 1]),
                     r=["IDB", "PF"], w=["DIAG"])
            for (p0, n, q0) in cblocks:
                bank = nb_ % 4
                nb_ += 1
                for k in range(31):
                    P.op("pe", lambda e, k=k: e.matmul(PS[:, bank, :n], lhsT=DIAG[:, k, :], rhs=APAD[:, mc, p0 - 15 + k:p0 - 15 + k + n],
                                                      start=(k == 0), stop=(k == 30)), r=["DIAG", "APAD"], w=[("PS", bank)])
                P.op("dve", lambda e: e.tensor_scalar(out=AOUT[:, mc, q0:q0 + n], in0=PS[:, bank, :n], scalar1=CAB[:, mc:mc + 1], scalar2=None, op0=ALU.add),
                     r=[("PS", bank), "PF"], w=[("AOUT", q0)])
        for (p0, n, q0) in cblocks:
            ar = [("AOUT", q0)]
            P.op("act", lambda e: e.activation(out=SQ2[:, :, :n], in_=AOUT[:, :, q0:q0 + n], func=AF.Square), r=ar, w=["SQ2"])
            for mc in range(4):
                P.op("pe", lambda e, mc=mc: e.matmul(PS[:, 4, :n], lhsT=self.ONES, rhs=AOUT[:, mc, q0:q0 + n], start=(mc == 0), stop=(mc == 3)),
                     r=ar + ["ONES"], w=[("PS", 4)])
            for mc in range(4):
                P.op("pe", lambda e, mc=mc: e.matmul(PS[:, 5, :n], lhsT=self.ONES, rhs=SQ2[:, mc, :n], start=(mc == 0), stop=(mc == 3)),
                     r=["SQ2", "ONES"], w=[("PS", 5)])
            P.op("act", lambda e: e.activation(out=MEAN[:, :n], in_=PS[:, 4, :n], func=AF.Copy, scale=1.0 / 512), r=[("PS", 4)], w=["MEAN"])
            P.op("dve", lambda e: e.tensor_tensor(out=TA[:, :n], in0=MEAN[:, :n], in1=MEAN[:, :n], op=ALU.mult), r=["MEAN"], w=["TA"])
            P.op("dve", lambda e: e.scalar_tensor_tensor(out=VAR[:, :n], in0=PS[:, 5, :n], scalar=1.0 / 512, in1=TA[:, :n],
                                                         op0=ALU.mult, op1=ALU.subtract), r=[("PS", 5), "TA"], w=["VAR"])
            P.op("dve", lambda e: e.tensor_scalar(out=VAR[:, :n], in0=VAR[:, :n], scalar1=EPS, scalar2=None, op0=ALU.add), r=["VAR"], w=["VAR"])
            P.op("act", lambda e: e.activation(out=VAR[:, :n], in_=VAR[:, :n], func=AF.Sqrt), r=["VAR"], w=["VAR"])
            P.op("dve", lambda e: e.reciprocal(out=VAR[:, :n], in_=VAR[:, :n]), r=["VAR"], w=["VAR"])
            for mc in range(4):
                P.op("dve", lambda e, mc=mc: e.tensor_tensor(out=TA[:, :n], in0=AOUT[:, mc, q0:q0 + n], in1=MEAN[:, :n], op=ALU.subtract),
                     r=ar + ["MEAN"], w=["TA"])
                P.op("dve", lambda e, mc=mc: e.tensor_tensor(out=TA[:, :n], in0=TA[:, :n], in1=VAR[:, :n], op=ALU.mult),
                     r=["TA", "VAR"], w=["TA"])
                P.op("act", lambda e, mc=mc: e.activation(out=AOUT[:, mc, q0:q0 + n], in_=TA[:, :n], func=AF.Silu,
                                                         bias=LB[:, mc:mc + 1], scale=LG[:, mc:mc + 1]), r=["TA", "PF"], w=ar)
            for mc in range(4):
                P.op("dve", lambda e, mc=mc: e.tensor_scalar(out=C3a[:, :n], in0=GCH[:, mc, p0 - 1:p0 - 1 + n], scalar1=CBW[:, mc, 0:1], scalar2=None, op0=ALU.mult),
                     r=["GCH", "PF"], w=["C3a"])
                for k in (1, 2):
                    P.op("dve", lambda e, mc=mc, k=k: e.scalar_tensor_tensor(out=C3a[:, :n], in0=GCH[:, mc, p0 - 1 + k:p0 - 1 + k + n], scalar=CBW[:, mc, k:k + 1],
                                                                         in1=C3a[:, :n], op0=ALU.mult, op1=ALU.add), r=["GCH", "PF", "C3a"], w=["C3a"])
                P.op("pool", lambda e, mc=mc: e.tensor_tensor(out=GB[:, mc, q0:q0 + n], in0=GB[:, mc, q0:q0 + n], in1=C3a[:, :n], op=ALU.mult),
                     r=["C3a", "GB"], w=["GB"])
        self.barrier()
        n = 0
        for b in range(5):
            c0, c1 = BLOCKS[b]
            nb = c1 - c0
            for mc in range(DC):
                bank = n % 4
                n += 1
                for k in range(DC):
                    src = AOUT[:, k, c0:c1] if k < 4 else GB[:, k - 4, c0:c1]
                    P.op("pe", lambda e, k=k, src=src, mc=mc, bank=bank, nb=nb: e.matmul(
                        PS[:, bank, :nb], lhsT=WO[:, k, mc * 128:(mc + 1) * 128], rhs=src, start=(k == 0), stop=(k == DC - 1)),
                        r=["WO", "GB"] + [("AOUT", q) for q in (0, 512, 1024, 1536, 2048, NLAT)], w=[("PS", bank)])
                for (s0, s1, s) in segs(c0, c1):
                    P.op("dve", lambda e, mc=mc, bank=bank, s0=s0, s1=s1, s=s, c0=c0: e.scalar_tensor_tensor(
                        out=XT[:, mc, s0:s1], in0=PS[:, bank, s0 - c0:s1 - c0], scalar=self.mod(lslot, 2, s, mc), in1=XT[:, mc, s0:s1],
                        op0=ALU.mult, op1=ALU.add), r=[("PS", bank), ("XT", b), "MOD"], w=[("XT", b)])
        self.barrier()

    def moe(self, lslot, l):
        P, PS, XT, A = self.P, self.PS, self.XT, self.arena
        last = (l == 3)
        MB_ = [(HALO + i * 512, HALO + (i + 1) * 512) for i in range(4)] if last else BLOCKS
        NB_ = len(MB_)
        nl = len(self.layers)
        wge = self.din("moe_wge", [nl, 1024, 20])
        w1 = self.din("moe_w1", [nl, 16, 1024, 512])
        w3 = self.din("moe_w3", [nl, 16, 1024, 512])
        w2 = self.din("moe_w2", [nl, 16, 512, 1024])
        off = OFF_S
        T, off = self.carve(off, [128, DC, NT], BF16)
        W1, W3, W2 = [], [], []
        for i in range(2):
            t, off = self.carve(off, [128, DC, 512], BF16); W1.append(t)
            t, off = self.carve(off, [128, DC, 512], BF16); W3.append(t)
            t, off = self.carve(off, [128, 4, 1024], BF16); W2.append(t)
        GTH, off = self.carve(off, [16, NT], BF16)
        GTL, off = self.carve(off, [16, NT], BF16)
        WGE, off = self.carve(off, [128, DC, 20], BF16)
        RT, off = self.carve(off, [128, 176], F32)
        offX = off
        self.norm_scratch(offX)
        def load_expert(ex):
            sl = ex % 2
            P.dma("pool", W1[sl], w1[lslot, ex].rearrange("(c p) m -> p c m", p=128), w=[("W1", sl)])
            P.dma("pool", W3[sl], w3[lslot, ex].rearrange("(c p) m -> p c m", p=128), w=[("W3", sl)])
            P.dma("pool", W2[sl], w2[lslot, ex].rearrange("(c p) m -> p c m", p=128), w=[("W2", sl)])
        load_expert(0)
        load_expert(1)
        for b in range(NB_):
            self.norm_mod(b, lambda c, s0, s1: T[:, c, s0:s1], lslot, 4, 3, [("T", b)], rng=MB_[b], psum_rs=True)
        P.dma("pool", WGE, wge[lslot].rearrange("(c p) m -> p c m", p=128), w=["WGE"])
        self.barrier()
        o, _ = PFM["moe_b"]
        MB = A[:, OFF_PF + o + l * 20:OFF_PF + o + (l + 1) * 20]
        NTL = 16 if last else NT // 128
        tbase = HALO if last else 0
        ro = offX
        LGr, ro = self.carve(ro, [128, NTL, 20], F32)
        GOHr, ro = self.carve(ro, [128, NTL, 4], F32)
        GEXr, ro = self.carve(ro, [128, NTL, 4], F32)
        EMr, ro = self.carve(ro, [128, NTL, 16], F32)
        OH1r, ro = self.carve(ro, [128, NTL, 16], F32)
        OH2r, ro = self.carve(ro, [128, NTL, 16], F32)
        GAr, ro = self.carve(ro, [128, NTL, 16], F32)
        GHFr, ro = self.carve(ro, [128, NTL, 16], F32)
        GLOr, ro = self.carve(ro, [128, NTL, 16], F32)
        GHBr, ro = self.carve(ro, [128, NTL, 16], BF16)
        SCr, ro = self.carve(ro, [128, 8, NTL], F32)
        assert ro <= AW
        GMv, GWv, M1v, M2v, E2v, W1v = SCr[:, 0, :], SCr[:, 1, :], SCr[:, 2, :], SCr[:, 3, :], SCr[:, 4, :], SCr[:, 5, :]
        BIG = 1.0e30
        X_ = mybir.AxisListType.X
        rt = ["RT"]
        for t in range(NTL):
            b = min(t // 4, 4)
            tc0 = tbase + t * 128
            for c in range(DC):
                P.op("pe", lambda e, c=c: e.matmul(PS[:, 6, t * 20:(t + 1) * 20], lhsT=T[:, c, tc0:tc0 + 128], rhs=WGE[:, c, :],
                                                  start=(c == 0), stop=(c == DC - 1)), r=[("T", b), "WGE"], w=[("PS", 6)])
        PSV = PS[:, 6, 0:NTL * 20].rearrange("p (t k) -> p t k", k=20)
        for jj in range(20):
            P.op("dve", lambda e, jj=jj: e.tensor_scalar(out=LGr[:, :, jj], in0=PSV[:, :, jj], scalar1=MB[:, jj:jj + 1], scalar2=None, op0=ALU.add),
                 r=[("PS", 6), "PF"], w=rt)
        P.op("dve", lambda e: e.reduce_max(out=GMv, in_=LGr[:, :, 0:4], axis=X_), r=rt, w=rt)
        for g in range(4):
            P.op("dve", lambda e, g=g: e.tensor_tensor(out=GOHr[:, :, g], in0=LGr[:, :, g], in1=GMv, op=ALU.is_ge), r=rt, w=rt)
            P.op("dve", lambda e, g=g: e.tensor_tensor(out=GEXr[:, :, g], in0=LGr[:, :, g], in1=GMv, op=ALU.subtract), r=rt, w=rt)
        P.op("act", lambda e: e.activation(out=GEXr, in_=GEXr, func=AF.Exp), r=rt, w=rt)
        P.op("dve", lambda e: e.reduce_sum(out=GWv, in_=GEXr, axis=X_), r=rt, w=rt)
        P.op("dve", lambda e: e.reciprocal(out=GWv, in_=GWv), r=rt, w=rt)
        P.op("dve", lambda e: e.tensor_scalar(out=GOHr, in0=GOHr, scalar1=-1.0, scalar2=BIG, op0=ALU.add, op1=ALU.mult), r=rt, w=rt)
        for g in range(4):
            for i in range(4):
                jj = g * 4 + i
                P.op("dve", lambda e, g=g, jj=jj: e.tensor_tensor(out=EMr[:, :, jj], in0=LGr[:, :, 4 + jj], in1=GOHr[:, :, g], op=ALU.add), r=rt, w=rt)
        P.op("dve", lambda e: e.reduce_max(out=M1v, in_=EMr, axis=X_), r=rt, w=rt)
        for jj in range(16):
            P.op("dve", lambda e, jj=jj: e.tensor_tensor(out=OH1r[:, :, jj], in0=EMr[:, :, jj], in1=M1v, op=ALU.is_ge), r=rt, w=rt)
        P.op("dve", lambda e: e.scalar_tensor_tensor(out=EMr, in0=OH1r, scalar=-BIG, in1=EMr, op0=ALU.mult, op1=ALU.add), r=rt, w=rt)
        P.op("dve", lambda e: e.reduce_max(out=M2v, in_=EMr, axis=X_), r=rt, w=rt)
        for jj in range(16):
            P.op("dve", lambda e, jj=jj: e.tensor_tensor(out=OH2r[:, :, jj], in0=EMr[:, :, jj], in1=M2v, op=ALU.is_ge), r=rt, w=rt)
        P.op("dve", lambda e: e.tensor_tensor(out=E2v, in0=M2v, in1=M1v, op=ALU.subtract), r=rt, w=rt)
        P.op("act", lambda e: e.activation(out=E2v, in_=E2v, func=AF.Exp), r=rt, w=rt)
        P.op("dve", lambda e: e.tensor_scalar(out=W1v, in0=E2v, scalar1=1.0, scalar2=None, op0=ALU.add), r=rt, w=rt)
        P.op("dve", lambda e: e.reciprocal(out=W1v, in_=W1v), r=rt, w=rt)
        P.op("dve", lambda e: e.tensor_tensor(out=E2v, in0=E2v, in1=W1v, op=ALU.mult), r=rt, w=rt)
        P.op("dve", lambda e: e.tensor_tensor(out=W1v, in0=W1v, in1=GWv, op=ALU.mult), r=rt, w=rt)
        P.op("dve", lambda e: e.tensor_tensor(out=E2v, in0=E2v, in1=GWv, op=ALU.mult), r=rt, w=rt)
        for jj in range(16):
            P.op("dve", lambda e, jj=jj: e.tensor_tensor(out=GAr[:, :, jj], in0=OH1r[:, :, jj], in1=W1v, op=ALU.mult), r=rt, w=rt)
            P.op("dve", lambda e, jj=jj: e.tensor_tensor(out=OH2r[:, :, jj], in0=OH2r[:, :, jj], in1=E2v, op=ALU.mult), r=rt, w=rt)
        P.op("dve", lambda e: e.tensor_tensor(out=GAr, in0=GAr, in1=OH2r, op=ALU.add), r=rt, w=rt)
        P.op("dve", lambda e: e.tensor_copy(out=GHBr, in_=GAr), r=rt, w=rt)
        P.op("dve", lambda e: e.tensor_copy(out=GHFr, in_=GHBr), r=rt, w=rt)
        P.op("dve", lambda e: e.tensor_tensor(out=GLOr, in0=GAr, in1=GHFr, op=ALU.subtract), r=rt, w=rt)
        for t0_ in range(0, NTL, 4):
            nt_ = min(4, NTL - t0_)
            for tt in range(nt_):
                t = t0_ + tt
                P.op("pe", lambda e, t=t, tt=tt: e.transpose(out=PS[0:16, 5, tt * 128:(tt + 1) * 128], in_=GHFr[:, t, :], identity=self.ID), r=rt + ["PF"], w=[("PS", 5)])
                P.op("pe", lambda e, t=t, tt=tt: e.transpose(out=PS[0:16, 4, tt * 128:(tt + 1) * 128], in_=GLOr[:, t, :], identity=self.ID), r=rt + ["PF"], w=[("PS", 4)])
            c0g = tbase + t0_ * 128
            gres = [("GT", bb) for bb in range(NB_)]
            P.op("act", lambda e: e.activation(out=GTH[:, c0g:c0g + nt_ * 128], in_=PS[0:16, 5, 0:nt_ * 128], func=AF.Copy), r=[("PS", 5)], w=gres)
            P.op("act", lambda e: e.activation(out=GTL[:, c0g:c0g + nt_ * 128], in_=PS[0:16, 4, 0:nt_ * 128], func=AF.Copy), r=[("PS", 4)], w=gres)
        self.barrier()
        off = offX
        HID, SGs, HGs, GBC = [], [], [], []
        for i in range(2):
            t, off = self.carve(off, [128, 4, 512], BF16); HID.append(t)
        for i in range(2):
            t, off = self.carve(off, [128, 512], F32); SGs.append(t)
            t, off = self.carve(off, [128, 512], F32); HGs.append(t)
        for i in range(2):
            t, off = self.carve(off, [128, 512], F32); GBC.append(t)
        assert off <= AW, (off, AW)
        nh = 0
        no = 0
        def h_phase(ex, b, hb):
            nonlocal nh
            sl = ex % 2
            c0, c1 = MB_[b]
            nb = c1 - c0
            gbc = GBC[hb]
            P.op("pe", lambda e: e.matmul(PS[:, 6, :nb], lhsT=self.SEL[:, ex, :], rhs=GTH[:, c0:c1], start=True, stop=False),
                 r=["SEL", ("GT", b)], w=[("PS", 6)])
            P.op("pe", lambda e: e.matmul(PS[:, 6, :nb], lhsT=self.SEL[:, ex, :], rhs=GTL[:, c0:c1], start=False, stop=True),
                 r=["SEL", ("GT", b)], w=[("PS", 6)])
            P.op("act", lambda e: e.activation(out=gbc[:, :nb], in_=PS[:, 6, :nb], func=AF.Copy), r=[("PS", 6)], w=[("GBC", hb)])
            for hc in range(4):
                i2 = nh % 2
                nh += 1
                b1, b3 = i2, 2 + i2
                for k in range(DC):
                    P.op("pe", lambda e, k=k: e.matmul(PS[:, b1, :nb], lhsT=W1[sl][:, k, hc * 128:(hc + 1) * 128], rhs=T[:, k, c0:c1],
                                                      start=(k == 0), stop=(k == DC - 1)), r=[("W1", sl), ("T", b)], w=[("PS", b1)])
                for k in range(DC):
                    P.op("pe", lambda e, k=k: e.matmul(PS[:, b3, :nb], lhsT=W3[sl][:, k, hc * 128:(hc + 1) * 128], rhs=T[:, k, c0:c1],
                                                      start=(k == 0), stop=(k == DC - 1)), r=[("W3", sl), ("T", b)], w=[("PS", b3)])
                sg, hg = SGs[i2], HGs[i2]
                P.op("act", lambda e: e.activation(out=sg[:, :nb], in_=PS[:, b1, :nb], func=AF.Silu), r=[("PS", b1)], w=[("SGs", i2)])
                P.op("dve", lambda e: e.tensor_tensor(out=hg[:, :nb], in0=sg[:, :nb], in1=PS[:, b3, :nb], op=ALU.mult),
                     r=[("SGs", i2), ("PS", b3)], w=[("HGs", i2)])
                P.op("pool", lambda e: e.tensor_tensor(out=HID[hb][:, hc, :nb], in0=hg[:, :nb], in1=gbc[:, :nb], op=ALU.mult),
                     r=[("HGs", i2), ("GBC", hb)], w=[("HID", hb)])

        def w2_phase(ex, b, hb):
            nonlocal no
            sl = ex % 2
            c0, c1 = MB_[b]
            nb = c1 - c0
            for fc in range(DC):
                bo = 4 + no % 2
                no += 1
                for hc in range(4):
                    P.op("pe", lambda e, hc=hc: e.matmul(PS[:, bo, :nb], lhsT=W2[sl][:, hc, fc * 128:(fc + 1) * 128], rhs=HID[hb][:, hc, :nb],
                                                        start=(hc == 0), stop=(hc == 3)), r=[("W2", sl), ("HID", hb)], w=[("PS", bo)])
                for (s0, s1, s) in segs(c0, c1):
                    P.op("dve", lambda e: e.scalar_tensor_tensor(
                        out=XT[:, fc, s0:s1], in0=PS[:, bo, s0 - c0:s1 - c0], scalar=self.mod(lslot, 5, s, fc), in1=XT[:, fc, s0:s1],
                        op0=ALU.mult, op1=ALU.add), r=[("PS", bo), ("XT", b), "MOD"], w=[("XT", b)])

        its = [(ex, b) for ex in range(16) for b in range(NB_)]
        prev = None
        for n_it, (ex, b) in enumerate(its):
            hb = n_it % 2
            h_phase(ex, b, hb)
            if prev is not None:
                w2_phase(*prev)
            prev = (ex, b, hb)
            if b == NB_ - 1 and ex + 2 < 16:
                pending = ex + 2
            if b == 0 and ex >= 1 and ex + 1 < 16:
                load_expert(ex + 1)
        w2_phase(*prev)
        self.barrier()

    def odd_mixer(self, lslot, l):
        P, PS, XT, A, nc = self.P, self.PS, self.XT, self.arena, self.nc
        j = l // 2
        ods = [x for x in self.layers if x % 2 == 1]
        nod = len(ods)
        js = ods.index(l)
        ctx_out = (l != 3)
        w_in = self.din("od_w_in", [nod, 1024, 1344])
        w_uq = self.din("od_w_uq", [nod, 384, 768])
        w_uqs = self.din("od_w_uqs", [nod, 384, 768])
        w_ukv = self.din("od_w_ukv", [nod, 256, 1024])
        w_pool = self.din("od_w_pool", [nod, 4, 128, 128])
        w_out = self.din("od_w_out", [nod, 1024, 1024])
        rope_d = self.din("rope", [128, 2, NLAT])
        XSP = nc.dram_tensor("xsp%d" % l, [128, DC * NT], F32).ap().rearrange("p (a b) -> p a b", a=DC)
        XI = nc.dram_tensor("xi%d" % l, [256, NOWN], BF16)
        XO = nc.dram_tensor("xo%d" % l, [4 * 256, NOWN], BF16)
        XI2 = nc.dram_tensor("xj%d" % l, [256, NOWN], BF16)
        XO2 = nc.dram_tensor("xp%d" % l, [4 * 256, NOWN], BF16)
        cck = "cc%d" % l
        P.sems[cck] = nc.alloc_semaphore(cck)
        P.semval[cck] = 0
        cck2 = "cd%d" % l
        P.sems[cck2] = nc.alloc_semaphore(cck2)
        P.semval[cck2] = 0
        off = OFF_S
        QN, off = self.carve(off, [128, 3, NT], BF16)
        KVN, off = self.carve(off, [128, 2, NT], BF16)
        KRT, off = self.carve(off, [128, NT], BF16)
        ROPE, off = self.carve(off, [128, 2, NLAT], F32)
        UP, off = self.carve(off, [128, 4, NPAD], BF16)
        offL = off
        HLB, off = self.carve(off, [128, DC, 512], BF16)
        WIN, off = self.carve(off, [128, DC, 1344], BF16)
        off = self.norm_scratch(off)
        QC, off = self.carve(off, [128, 3, 512], F32)
        assert off <= AW, (off, AW)
        MASK = self.pf("mask")
        o, _ = PFM["q_g"]
        QG = A[:, OFF_PF + o + j * 3:OFF_PF + o + j * 3 + 3]
        o, _ = PFM["kv_g"]
        KG = A[:, OFF_PF + o + j * 2:OFF_PF + o + j * 2 + 2]
        SQ, RS = self.nSQ, self.nRS
        rot = [0]
        def nbank(lo=0, n=6):
            rot[0] += 1
            return lo + rot[0] % n
        P.dma("pool", WIN, w_in[js].rearrange("(c p) m -> p c m", p=128), w=["WIN"])
        P.dma("sp", ROPE, rope_d, w=["ROPE"])
        P.op("pool", lambda e: e.memset(UP, 0.0), w=["UP"])
        P.op("pool", lambda e: e.memset(KRT, 0.0), w=["KRT"])
        for b in range(5):
            c0, c1 = BLOCKS[b]
            nb = c1 - c0
            self.norm_mod(b, lambda c, s0, s1: HLB[:, c, s0 - c0:s1 - c0], lslot, 1, 0, ["HLB"])
            def mm(col0, m, bank, rows=128):
                for k in range(DC):
                    P.op("pe", lambda e, k=k: e.matmul(PS[0:m, bank, :nb], lhsT=WIN[:, k, col0:col0 + m], rhs=HLB[:, k, :nb],
                                                      start=(k == 0), stop=(k == DC - 1)), r=["WIN", "HLB"], w=[("PS", bank)])
            for (base, nch, DSTN, G, dim, res) in ((0, 3, QN, QG, 384, "QN"), (384, 2, KVN, KG, 256, "KVN")):
                for i in range(nch):
                    bank = nbank()
                    mm(base + i * 128, 128, bank)
                    P.op("act", lambda e, i=i, bank=bank: e.activation(out=QC[:, i, :nb], in_=PS[:, bank, :nb], func=AF.Copy), r=[("PS", bank)], w=["QC"])
                    P.op("act", lambda e, i=i, bank=bank: e.activation(out=SQ[:, i, :nb], in_=PS[:, bank, :nb], func=AF.Square), r=[("PS", bank)], w=["nSQ"])
                for i in range(nch):
                    P.op("pe", lambda e, i=i: e.matmul(PS[:, 7, :nb], lhsT=self.ONES, rhs=SQ[:, i, :nb], start=(i == 0), stop=(i == nch - 1)),
                         r=["nSQ", "ONES"], w=[("PS", 7)])
                P.op("dve", lambda e: e.tensor_scalar(out=RS[:, :nb], in0=PS[:, 7, :nb], scalar1=1.0 / dim, scalar2=EPS, op0=ALU.mult, op1=ALU.add),
                     r=[("PS", 7)], w=["nRS"])
                P.op("act", lambda e: e.activation(out=RS[:, :nb], in_=RS[:, :nb], func=AF.Sqrt), r=["nRS"], w=["nRS"])
                P.op("dve", lambda e: e.reciprocal(out=RS[:, :nb], in_=RS[:, :nb]), r=["nRS"], w=["nRS"])
                for i in range(nch):
                    P.op("dve", lambda e, i=i: e.scalar_tensor_tensor(out=DSTN[:, i, c0:c1], in0=QC[:, i, :nb], scalar=G[:, i:i + 1], in1=RS[:, :nb],
                                                                     op0=ALU.mult, op1=ALU.mult), r=["QC", "nRS", "PF"], w=[res])
            for g in range(4):
                bank = nbank()
                mm(640 + g * 128, 128, bank)
                for (s0, s1, s) in segs(c0, c1):
                    P.op("act", lambda e, g=g, bank=bank, s0=s0, s1=s1: e.activation(out=UP[:, g, pcol(s0):pcol(s0) + s1 - s0], in_=PS[:, bank, s0 - c0:s1 - c0],
                                                                                    func=AF.Copy), r=[("PS", bank)], w=["UP"])
            bA = nbank()
            mm(1152, 96, bA)
            bB = nbank()
            mm(1248, 96, bB)
            for (s0, s1, s) in segs(c0, c1):
                n = s1 - s0
                if s == 0:
                    T1, T2 = self.nTM[0], self.nTM[1]
                    P.op("dve", lambda e: e.tensor_tensor(out=T1[64:96, :n], in0=PS[64:96, bA, s0 - c0:s1 - c0], in1=ROPE[64:96, 0, s0:s1], op=ALU.mult),
                         r=[("PS", bA), "ROPE"], w=[("nTM", 0)])
                    P.op("dve", lambda e: e.tensor_tensor(out=T2[64:96, :n], in0=PS[64:96, bB, s0 - c0:s1 - c0], in1=ROPE[64:96, 1, s0:s1], op=ALU.mult),
                         r=[("PS", bB), "ROPE"], w=[("nTM", 1)])
                    P.op("pool", lambda e: e.tensor_tensor(out=KRT[64:96, s0:s1], in0=T1[64:96, :n], in1=T2[64:96, :n], op=ALU.add),
                         r=[("nTM", 0), ("nTM", 1)], w=["KRT"])
                else:
                    P.op("act", lambda e: e.activation(out=KRT[64:96, s0:s1], in_=PS[64:96, bA, s0 - c0:s1 - c0], func=AF.Copy), r=[("PS", bA), ("PS", bB)], w=["KRT"])
        for g in range(4):
            for (pc0, m0) in ((PADW, 0), (PADW + NLAT - HALO, 64)):
                P.op("pool", lambda e, g=g, pc0=pc0, m0=m0: e.tensor_tensor(out=UP[:, g, pc0:pc0 + HALO], in0=UP[:, g, pc0:pc0 + HALO],
                                                                           in1=MASK[:, m0:m0 + HALO], op=ALU.mult), r=["UP", "PF"], w=["UP"])
        self.barrier()
        XIa, XOa = XI.ap(), XO.ap()
        for c in range(2):
            P.dma("sp", XIa[c * 128:(c + 1) * 128, :], KVN[:, c, HALO:HALO + NOWN], r=["KVN"], w=["XI"])
        XI2a, XO2a = XI2.ap(), XO2.ap()
        P.dma("sp", XI2a[0:128, :], KRT[:, HALO:HALO + NOWN], r=["KRT"], w=["XI2"])
        P.dma("sp", XI2a[128:256, :], KRT[:, HALO:HALO + NOWN], r=["KRT"], w=["XI2"])
        off = offL
        WO, off = self.carve(off, [128, DC, 1024], BF16)
        WPL, off = self.carve(off, [128, 4, 128], BF16)
        LA, off = self.carve(off, [128, NPAD], F32)
        LB, off = self.carve(off, [128, NPAD], F32)
        YP, off = self.carve(off, [128, 4, 512], BF16)
        BS, off = self.carve(off, [128, 4], F32)
        assert off <= AW
        P.dma("pool", WO, w_out[js].rearrange("(c p) m -> p c m", p=128), w=["WO"])
        P.dma("pool", WPL, w_pool[js].rearrange("g c d -> c g d"), w=["WPL"])
        o, _ = PFM["b_pool"]
        BP = A[:, OFF_PF + o + j * 4:OFF_PF + o + j * 4 + 4]
        o, _ = PFM["s_pool"]
        SP_ = A[:, OFF_PF + o + j * 4:OFF_PF + o + j * 4 + 4]
        P.op("dve", lambda e: e.tensor_tensor(out=BS, in0=BP, in1=SP_, op=ALU.mult), r=["PF"], w=["BS"])
        o, _ = PFM["pcorr_lat"]
        PCL = A[:, OFF_PF + o:OFF_PF + o + 128].rearrange("p (g s) -> p g s", g=4)
        o, _ = PFM["pcorr_ctx"]
        PCC = A[:, OFF_PF + o:OFF_PF + o + 128].rearrange("p (g s) -> p g s", g=4)
        for g in range(4):
            src = UP[:, g, :]
            P.op("pool", lambda e: e.tensor_tensor(out=LA[:, 1:NPAD], in0=src[:, 0:NPAD - 1], in1=src[:, 1:NPAD], op=ALU.add), r=["UP"], w=["LA"])
            cur, oth, cn, on = LA, LB, "LA", "LB"
            lo, hi = 1, NPAD
            for lev in range(1, g + 1):
                d = 1 << (lev - 1)
                lo, hi = lo + d, hi - d
                P.op("pool", lambda e, cur=cur, oth=oth, d=d, lo=lo, hi=hi: e.tensor_tensor(out=oth[:, lo:hi], in0=cur[:, lo - d:hi - d], in1=cur[:, lo + d:hi + d], op=ALU.add),
                     r=[cn], w=[on])
                cur, oth, cn, on = oth, cur, on, cn
            for (pc, tab, t0) in ((PADW + HALO - 8, PCL, 0), (PADW + NLAT - HALO - 8, PCL, 16), (pcol(NLAT), PCC, 0), (pcol(NLAT) + NCTX - 16, PCC, 16)):
                P.op("pool", lambda e, cur=cur, pc=pc, tab=tab, t0=t0, g=g: e.tensor_tensor(out=cur[:, pc:pc + 16], in0=cur[:, pc:pc + 16], in1=tab[:, g, t0:t0 + 16], op=ALU.mult),
                     r=[cn, "PF"], w=[cn])
            w = 2 << g
            P.op("dve", lambda e, cur=cur, g=g, w=w: e.scalar_tensor_tensor(out=UP[:, g, PADW:NPAD - PADW], in0=cur[:, PADW:NPAD - PADW], scalar=1.0 / w,
                                                                       in1=UP[:, g, PADW:NPAD - PADW], op0=ALU.mult, op1=ALU.subtract), r=[cn, "UP"], w=["UP"])
        P.custom("pool", lambda e: e.collective_compute("AllGather", ALU.bypass, replica_groups=[[0, 1, 2, 3], [4, 5, 6, 7]],
                                                        ins=[XI.ap().opt()], outs=[XO.ap().opt()]), (cck, None), r=["XI"], w=["XO"])
        P.wait_all("pool", ["XO"])
        P.custom("pool", lambda e: e.collective_compute("AllGather", ALU.bypass, replica_groups=[[0, 1, 2, 3], [4, 5, 6, 7]],
                                                        ins=[XI2.ap().opt()], outs=[XO2.ap().opt()]), (cck2, None), r=["XI2"], w=["XO2"])
        P.wait_all("pool", ["XO2"])
        for b in range(5):
            c0, c1 = BLOCKS[b]
            nb = c1 - c0
            for g in range(4):
                bank = nbank()
                for (s0, s1, s) in segs(c0, c1):
                    P.op("pe", lambda e, g=g, bank=bank, s0=s0, s1=s1: e.matmul(PS[:, bank, s0 - c0:s1 - c0], lhsT=WPL[:, g, :], rhs=UP[:, g, pcol(s0):pcol(s0) + s1 - s0],
                                                                            start=True, stop=True), r=["WPL", "UP"], w=[("PS", bank)])
                P.op("act", lambda e, g=g, bank=bank: e.activation(out=YP[:, g, :nb], in_=PS[:, bank, :nb], func=AF.Identity, bias=BS[:, g:g + 1], scale=SP_[:, g:g + 1]),
                     r=[("PS", bank), "BS", "PF"], w=["YP"])
            for mc in range(DC):
                bank = nbank()
                for g in range(4):
                    P.op("pe", lambda e, g=g, mc=mc, bank=bank: e.matmul(PS[:, bank, :nb], lhsT=WO[:, 4 + g, mc * 128:(mc + 1) * 128], rhs=YP[:, g, :nb],
                                                                     start=(g == 0), stop=(g == 3)), r=["WO", "YP"], w=[("PS", bank)])
                for (s0, s1, s) in segs(c0, c1):
                    P.op("dve", lambda e, mc=mc, bank=bank, s0=s0, s1=s1, s=s: e.scalar_tensor_tensor(
                        out=XT[:, mc, s0:s1], in0=PS[:, bank, s0 - c0:s1 - c0], scalar=self.mod(lslot, 2, s, mc), in1=XT[:, mc, s0:s1],
                        op0=ALU.mult, op1=ALU.add), r=[("PS", bank), ("XT", b), "MOD"], w=[("XT", b)])
        self.barrier()
        for b, (c0, c1) in enumerate(BLOCKS):
            P.dma("sp", XSP[:, :, c0:c1], XT[:, :, c0:c1], r=[("XT", b)], w=["XSP"])
        P.wait_all("sp", ["XSP"])
        self.barrier()
        offx = OFF_XT
        KVA, offx = self.carve(offx, [128, 2, NKEY], BF16)
        KB, offx = self.carve(offx, [128, NKEY], BF16)
        VB, offx = self.carve(offx, [128, NKT, 128], BF16)
        QB, offx = self.carve(offx, [128, NT], BF16)
        assert offx <= OFF_S
        for rr in range(4):
            for c in range(2):
                P.dma("sp", KVA[:, c, NCTX + rr * NOWN:NCTX + (rr + 1) * NOWN], XOa[rr * 256 + c * 128:rr * 256 + (c + 1) * 128, :], r=["XO"], w=["KVA"])
            P.dma("sp", KB[:, NCTX + rr * NOWN:NCTX + (rr + 1) * NOWN], XO2a[rr * 256:rr * 256 + 128, :], r=["XO2"], w=["KBR", "KB"])
        for c in range(2):
            P.op("pool", lambda e, c=c: e.tensor_copy(out=KVA[:, c, 0:NCTX], in_=KVN[:, c, NLAT:NT]), r=["KVN"], w=["KVA"])
        P.op("pool", lambda e: e.tensor_copy(out=KB[64:96, 0:NCTX], in_=KRT[64:96, NLAT:NT]), r=["KRT"], w=["KBR"])
        off = offL + DC * 1024 // 2
        WUQ, off = self.carve(off, [128, 3, 768], BF16)
        WUQS, off = self.carve(off, [128, 3, 768], BF16)
        WUKV, off = self.carve(off, [128, 2, 1024], BF16)
        PT = []
        for i in range(4):
            t, off = self.carve(off, [128, 512], BF16); PT.append(t)
        OSB, off = self.carve(off, [128, 512], F32)
        RCP, off = self.carve(off, [128, 512], F32)
        T1, off = self.carve(off, [128, 512], F32)
        T2, off = self.carve(off, [128, 512], F32)
        assert off <= AW
        ATT, _ = self.carve(offL - 4 * NPAD // 2, [128, 4, NT], BF16)
        P.op("pool", lambda e: e.memset(ATT, 0.0), w=["ATT"])
        P.dma("pool", WUQ, w_uq[js].rearrange("(c p) m -> p c m", p=128), w=["WUQ"])
        P.dma("pool", WUQS, w_uqs[js].rearrange("(c p) m -> p c m", p=128), w=["WUQS"])
        P.dma("pool", WUKV, w_ukv[js].rearrange("(c p) m -> p c m", p=128), w=["WUKV"])
        qblocks = [(0, 512), (512, 1024), (1024, 1536), (1536, 2048), (2048, NT)] if ctx_out else [(HALO + i * 512, HALO + (i + 1) * 512) for i in range(4)]
        kblocks = [(i * 512, min((i + 1) * 512, NKEY)) for i in range((NKEY + 511) // 512)]
        npt = [0]
        nob = [0]
        nev = [0]
        prot = [0]
        PB = (5, 6, 0, 1, 2, 7)
        def pbank():
            prot[0] += 1
            return PB[prot[0] % len(PB)]
        def evac(dst, src, r, w):
            nev[0] += 1
            if nev[0] % 2:
                P.op("act", lambda e: e.activation(out=dst, in_=src, func=AF.Copy), r=r, w=w)
            else:
                P.op("dve", lambda e: e.tensor_copy(out=dst, in_=src), r=r, w=w)
        for h in range(8):
            hp = h % 2
            for (k0, k1) in kblocks:
                bank = pbank()
                for c in range(2):
                    P.op("pe", lambda e, c=c: e.matmul(PS[0:64, bank, :k1 - k0], lhsT=WUKV[:, c, h * 128:h * 128 + 64], rhs=KVA[:, c, k0:k1],
                                                      start=(c == 0), stop=(c == 1)), r=["WUKV", "KVA"], w=[("PS", bank)])
                evac(KB[0:64, k0:k1], PS[0:64, bank, :k1 - k0], [("PS", bank)], ["KB"])
            P.op("pool", lambda e: e.memset(VB[:, :, (1 - hp) * 64:(1 - hp) * 64 + 64], 1.0), w=["VB"])
            for kt0 in range(0, NKT, 8):
                nk = min(8, NKT - kt0)
                bank = pbank()
                for jj in range(nk):
                    kt = kt0 + jj
                    for c in range(2):
                        P.op("pe", lambda e, c=c, jj=jj, kt=kt: e.matmul(PS[:, bank, jj * 64:(jj + 1) * 64], lhsT=KVA[:, c, kt * 128:(kt + 1) * 128],
                                                                        rhs=WUKV[:, c, h * 128 + 64:h * 128 + 128], start=(c == 0), stop=(c == 1)),
                             r=["WUKV", "KVA"], w=[("PS", bank)])
                evac(VB[:, kt0:kt0 + nk, hp * 64:hp * 64 + 64], PS[:, bank, 0:nk * 64].rearrange("p (a b) -> p a b", a=nk), [("PS", bank)], ["VB"])
            for (q0, q1) in qblocks:
                nq = q1 - q0
                bA = pbank()
                for c in range(3):
                    P.op("pe", lambda e, c=c: e.matmul(PS[0:96, bA, :nq], lhsT=WUQ[:, c, h * 96:(h + 1) * 96], rhs=QN[:, c, q0:q1], start=(c == 0), stop=(c == 2)),
                         r=["WUQ", "QN"], w=[("PS", bA)])
                bB = pbank()
                for c in range(3):
                    P.op("pe", lambda e, c=c: e.matmul(PS[0:96, bB, :nq], lhsT=WUQS[:, c, h * 96:(h + 1) * 96], rhs=QN[:, c, q0:q1], start=(c == 0), stop=(c == 2)),
                         r=["WUQS", "QN"], w=[("PS", bB)])
                P.op("act", lambda e: e.activation(out=QB[0:64, q0:q1], in_=PS[0:64, bA, :nq], func=AF.Copy), r=[("PS", bA)], w=["QB"])
                for (s0, s1, s) in segs(q0, q1):
                    n = s1 - s0
                    if s == 0:
                        P.op("dve", lambda e: e.tensor_tensor(out=T1[64:96, :n], in0=PS[64:96, bA, s0 - q0:s1 - q0], in1=ROPE[64:96, 0, s0:s1], op=ALU.mult),
                             r=[("PS", bA), "ROPE"], w=["T1"])
                        P.op("dve", lambda e: e.tensor_tensor(out=T2[64:96, :n], in0=PS[64:96, bB, s0 - q0:s1 - q0], in1=ROPE[64:96, 1, s0:s1], op=ALU.mult),
                             r=[("PS", bB), "ROPE"], w=["T2"])
                        P.op("pool", lambda e: e.tensor_tensor(out=QB[64:96, s0:s1], in0=T1[64:96, :n], in1=T2[64:96, :n], op=ALU.add), r=["T1", "T2"], w=["QB"])
                    else:
                        P.op("act", lambda e: e.activation(out=QB[64:96, s0:s1], in_=PS[64:96, bA, s0 - q0:s1 - q0], func=AF.Copy), r=[("PS", bA), ("PS", bB)], w=["QB"])
            jobs = [(q0, min(q1, NLAT), 0, NKT) for (q0, q1) in qblocks]
            if ctx_out:
                jobs.append((NLAT, NT, 0, 2))
            for (q0, q1, kta, ktb) in jobs:
                nq = q1 - q0
                ob = 3 + nob[0] % 2
                nob[0] += 1
                SBK = (0, 1, 2, 7)
                def s_mm(kt):
                    sb = SBK[kt % 4]
                    P.op("pe", lambda e: e.matmul(PS[:, sb, :nq], lhsT=KB[0:96, kt * 128:(kt + 1) * 128], rhs=QB[0:96, q0:q1], start=True, stop=True),
                         r=["KB", "KBR", "QB"], w=[("PS", sb)])
                s_mm(kta)
                if kta + 1 < ktb:
                    s_mm(kta + 1)
                for kt in range(kta, ktb):
                    if kt + 2 < ktb:
                        s_mm(kt + 2)
                    sb = SBK[kt % 4]
                    pi = npt[0] % 4
                    npt[0] += 1
                    P.op("act", lambda e: e.activation(out=PT[pi][:, :nq], in_=PS[:, sb, :nq], func=AF.Exp, scale=MLA_SCALE), r=[("PS", sb)], w=[("PT", pi)])
                    P.op("pe", lambda e: e.matmul(PS[:, ob, :nq], lhsT=VB[:, kt, :], rhs=PT[pi][:, :nq], start=(kt == kta), stop=(kt == ktb - 1)),
                         r=["VB", ("PT", pi)], w=[("PS", ob)])
                nlo, dlo = hp * 64, (1 - hp) * 64
                P.op("dve", lambda e: e.reciprocal(out=RCP[nlo:nlo + 64, :nq], in_=PS[dlo:dlo + 64, ob, :nq]), r=[("PS", ob)], w=["RCP"])
                P.op("dve", lambda e: e.tensor_tensor(out=ATT[nlo:nlo + 64, h // 2, q0:q1], in0=PS[nlo:nlo + 64, ob, :nq], in1=RCP[nlo:nlo + 64, :nq], op=ALU.mult),
                     r=[("PS", ob), "RCP"], w=["ATT"])
        self.barrier()
        for b, (c0, c1) in enumerate(BLOCKS):
            P.dma("sp", XT[:, :, c0:c1], XSP[:, :, c0:c1], r=["XSP"], w=[("XT", b)])
        for b in range(5):
            c0, c1 = BLOCKS[b]
            nb = c1 - c0
            for mc in range(DC):
                bank = nbank(0, 4)
                for k in range(4):
                    P.op("pe", lambda e, k=k, mc=mc: e.matmul(PS[:, bank, :nb], lhsT=WO[:, k, mc * 128:(mc + 1) * 128], rhs=ATT[:, k, c0:c1], start=(k == 0), stop=(k == 3)),
                         r=["WO", "ATT"], w=[("PS", bank)])
                for (s0, s1, s) in segs(c0, c1):
                    P.op("dve", lambda e, mc=mc, s0=s0, s1=s1, s=s: e.scalar_tensor_tensor(
                        out=XT[:, mc, s0:s1], in0=PS[:, bank, s0 - c0:s1 - c0], scalar=self.mod(lslot, 2, s, mc), in1=XT[:, mc, s0:s1],
                        op0=ALU.mult, op1=ALU.add), r=[("PS", bank), ("XT", b), "MOD"], w=[("XT", b)])
        self.barrier()

    def final_store(self):
        P, PS, XT, A = self.P, self.PS, self.XT, self.arena
        out = self.dout("out", [NOWN, 1024])
        off = OFF_S
        off = self.norm_scratch(off)
        XN, off = self.carve(off, [128, DC, 512], F32)
        OT = []
        for i in range(2):
            t, off = self.carve(off, [128, 1024], F32); OT.append(t)
        o, _ = PFM["final_g"]
        FG = A[:, OFF_PF + o:OFF_PF + o + 8]
        SQ, RS = self.nSQ, self.nRS
        nt = 0
        for qb in range(4):
            c0 = HALO + qb * 512
            c1 = c0 + 512
            xr = [("XT", b) for b in range(5)]
            P.op("act", lambda e, c0=c0, c1=c1: e.activation(out=SQ, in_=XT[:, :, c0:c1], func=AF.Square), r=xr, w=["nSQ"])
            for c in range(DC):
                P.op("pe", lambda e, c=c: e.matmul(PS[:, 7, :], lhsT=self.ONES, rhs=SQ[:, c, :], start=(c == 0), stop=(c == DC - 1)),
                     r=["nSQ", "ONES"], w=[("PS", 7)])
            P.op("dve", lambda e: e.tensor_scalar(out=RS, in0=PS[:, 7, :], scalar1=1.0 / D, scalar2=EPS, op0=ALU.mult, op1=ALU.add), r=[("PS", 7)], w=["nRS"])
            P.op("act", lambda e: e.activation(out=RS, in_=RS, func=AF.Sqrt), r=["nRS"], w=["nRS"])
            P.op("dve", lambda e: e.reciprocal(out=RS, in_=RS), r=["nRS"], w=["nRS"])
            for c in range(DC):
                P.op("dve", lambda e, c=c, c0=c0, c1=c1: e.scalar_tensor_tensor(out=XN[:, c, :], in0=XT[:, c, c0:c1], scalar=FG[:, c:c + 1], in1=RS,
                                                                            op0=ALU.mult, op1=ALU.mult), r=xr + ["nRS", "PF"], w=["XN"])
            for tt in range(4):
                i = nt % 2
                nt += 1
                for half in range(2):
                    bank = 2 * i + half
                    for c4 in range(4):
                        c = half * 4 + c4
                        P.op("pe", lambda e, c=c, c4=c4, tt=tt, bank=bank: e.transpose(out=PS[:, bank, c4 * 128:(c4 + 1) * 128],
                                                                                   in_=XN[:, c, tt * 128:(tt + 1) * 128], identity=self.ID),
                             r=["XN", "PF"], w=[("PS", bank)])
                    if half == 0:
                        P.op("act", lambda e, i=i, bank=bank: e.activation(out=OT[i][:, 0:512], in_=PS[:, bank, :], func=AF.Copy), r=[("PS", bank)], w=[("OT", i)])
                    else:
                        P.op("dve", lambda e, i=i, bank=bank: e.tensor_copy(out=OT[i][:, 512:1024], in_=PS[:, bank, :]), r=[("PS", bank)], w=[("OT", i)])
                r0 = qb * 512 + tt * 128
                P.dma("sp", out[r0:r0 + 128, :], OT[i], r=[("OT", i)], w=["OUT"])
        P.wait_all("sp", ["OUT"])
        for q in ("act", "dve", "pe", "pool"):
            pass


def build_program(layers, in_mode, out_mode):
    B = Builder(layers, in_mode, out_mode)
    B.prologue()
    if in_mode == "raw":
        B.load_x_raw()
    else:
        B.load_x_state()
    B.modulation()
    for li, l in enumerate(B.layers):
        if l % 2 == 0:
            B.even_mixer(li, l)
        else:
            B.odd_mixer(li, l)
        B.moe(li, l)
    if out_mode == "final":
        B.final_store()
    else:
        B.store_x_state()
    B.P.run()
    return B


def core_inputs(inp, W, core, layers, B):
    b, r = core // 4, core % 4
    m = {}
    pf, rope = host_pf(inp, core)
    m["pf"] = pf
    names = set(B.dram.keys())
    if "x_in" in names:
        xe = np.zeros((NLAT, 1024), np.float32)
        lo, hi = r * NOWN - HALO, (r + 1) * NOWN + HALO
        slo, shi = max(lo, 0), min(hi, SEQ)
        xe[slo - lo:shi - lo] = inp["x"][b, slo:shi]
        m["x_in"] = xe
        m["ctx_in"] = np.ascontiguousarray(inp["ctx"][b], dtype=np.float32)
    if "rope" in names:
        m["rope"] = rope
    ev = [l // 2 for l in layers if l % 2 == 0]
    od = [l // 2 for l in layers if l % 2 == 1]
    for k in names:
        if k in ("pf", "x_in", "ctx_in", "rope", "st_in", "st_out", "out"):
            continue
        if k.startswith("ev_"):
            m[k] = np.ascontiguousarray(W[k][ev])
        elif k.startswith("od_"):
            m[k] = np.ascontiguousarray(W[k][od])
        else:
            m[k] = np.ascontiguousarray(W[k][list(layers)])
    return m


def state_from_ref(x, xc, core):
    b, r = core // 4, core % 4
    xe = np.zeros((NT, 1024), np.float32)
    lo, hi = r * NOWN - HALO, (r + 1) * NOWN + HALO
    slo, shi = max(lo, 0), min(hi, SEQ)
    xe[slo - lo:shi - lo] = x[b, slo:shi]
    xe[NLAT:] = xc[b]
    return np.ascontiguousarray(xe.reshape(NT, 8, 128).transpose(2, 1, 0)).reshape(128, 8 * NT)


_CACHE = {}


def kernel(**inputs):
    inp = {k: np.asarray(v) for k, v in inputs.items()}
    layers = [0, 1, 2, 3]
    if "prog" not in _CACHE:
        _CACHE["prog"] = build_program(layers, "raw", "final")
    B = _CACHE["prog"]
    W = host_weights(inp)
    maps = [core_inputs(inp, W, core, layers, B) for core in range(8)]
    res = run_bass_kernel_spmd(B.nc, maps, core_ids=list(range(8)))
    out = np.zeros((2, SEQ, D), np.float32)
    for core in range(8):
        b, r = core // 4, core % 4
        out[b, r * NOWN:(r + 1) * NOWN] = np.asarray(res.results[core]["out"], dtype=np.float32)
    return out
```

```python
import numpy as np
import concourse.bass as bass
import concourse.mybir as mybir
from concourse.bass_utils import run_bass_kernel_spmd

F32 = mybir.dt.float32
BF16 = mybir.dt.bfloat16
ALU = mybir.AluOpType
AF = mybir.ActivationFunctionType

D = 1024
DC = 8
HALO = 64
NOWN = 2048
NLAT = NOWN + 2 * HALO
NCTX = 256
NT = NLAT + NCTX
PADW = 16
NPAD = PADW + NLAT + PADW + NCTX + PADW
EPS = 1e-6
SEQ = 8192
NKEY = NCTX + SEQ
NKT = NKEY // 128
MLA_SCALE = 96 ** -0.5
BLOCKS = [(0, 512), (512, 1024), (1024, 1536), (1536, 2048), (2048, 2432)]


def pcol(c):
    return c + PADW if c < NLAT else c + 2 * PADW


def segs(c0, c1):
    out = []
    if c0 < NLAT:
        out.append((c0, min(c1, NLAT), 0))
    if c1 > NLAT:
        out.append((max(c0, NLAT), c1, 1))
    return out


class _Rec:
    def __init__(self):
        self.call = None

    def __getattr__(self, name):
        def f(*a, **k):
            self.call = (name, a, k)
            return self
        return f


def _eager(fn):
    rec = _Rec()
    fn(rec)
    name, a, k = rec.call
    return lambda e: getattr(e, name)(*a, **k)


class Prog:
    CE = ("pe", "act", "dve", "pool")

    def __init__(self, nc, n_dma_sems=10):
        self.nc = nc
        self.streams = {e: [] for e in ("pe", "act", "dve", "pool", "sp")}
        self.sems = {}
        self.semval = {}
        for e in self.CE:
            self.sems[e] = nc.alloc_semaphore("s_" + e)
            self.semval[e] = 0
        self.dma_sems = {}
        for q in ("sp", "pool"):
            self.dma_sems[q] = []
            for i in range(n_dma_sems):
                k = "d_%s%d" % (q, i)
                self.sems[k] = nc.alloc_semaphore(k)
                self.semval[k] = 0
                self.dma_sems[q].append(k)
        self.dma_rr = {"sp": 0, "pool": 0}
        self.known = {e: {} for e in self.streams}
        self.last_w = {}
        self.readers = {}
        self.nops = 0

    def _deps(self, eng, r, w):
        deps = {}
        def add(ev, raw):
            if ev is None:
                return
            k, v = ev
            if k == eng:
                if not raw or eng == "pe" or v < self.semval[eng] - 1:
                    return
            if deps.get(k, 0) < v:
                deps[k] = v
        for x in r:
            add(self.last_w.get(x), True)
        for x in w:
            add(self.last_w.get(x), False)
            for ev in self.readers.get(x, ()):
                add(ev, False)
        return deps

    def _emit_waits(self, eng, deps):
        kn = self.known[eng]
        for k, v in deps.items():
            if kn.get(k, 0) >= v:
                continue
            kn[k] = v
            sem = self.sems[k]
            self.streams[eng].append(("wait", sem, v))

    def _commit(self, ev, r, w):
        for x in r:
            self.readers.setdefault(x, []).append(ev)
        for x in w:
            self.last_w[x] = ev
            self.readers[x] = []

    def op(self, eng, fn, r=(), w=()):
        deps = self._deps(eng, r, w)
        self._emit_waits(eng, deps)
        self.semval[eng] += 1
        ev = (eng, self.semval[eng])
        self.streams[eng].append(("op", _eager(fn), self.sems[eng], 1))
        self._commit(ev, r, w)
        self.nops += 1
        return ev

    def dma(self, q, out, in_, r=(), w=()):
        sems = self.dma_sems[q]
        k = sems[self.dma_rr[q] % len(sems)]
        self.dma_rr[q] += 1
        deps = self._deps(q, r, w)
        deps[k] = max(deps.get(k, 0), self.semval[k])
        self._emit_waits(q, deps)
        self.semval[k] += 16
        ev = (k, self.semval[k])
        self.streams[q].append(("op", lambda e, o=out, i=in_: e.dma_start(out=o, in_=i), self.sems[k], 16))
        self._commit(ev, r, w)
        self.nops += 1
        return ev

    def custom(self, q, fn, semkey_inc, r=(), w=()):
        k, inc = semkey_inc
        deps = self._deps(q, r, w)
        deps[k] = max(deps.get(k, 0), self.semval[k])
        self._emit_waits(q, deps)
        self.semval[k] += (inc if inc else 1)
        ev = (k, self.semval[k])
        self.streams[q].append(("op", _eager(fn), self.sems[k], inc))
        self._commit(ev, r, w)
        return ev

    def wait_all(self, q, res):
        deps = self._deps(q, res, res)
        self._emit_waits(q, deps)

    def run(self):
        nc = self.nc
        engs = {"pe": "tensor", "act": "scalar", "dve": "vector", "pool": "gpsimd", "sp": "sync"}
        with nc.Block() as block:
            for name, attr in engs.items():
                stream = self.streams[name]
                def body(e, stream=stream):
                    for it in stream:
                        if it[0] == "wait":
                            e.wait_ge(it[1], it[2])
                        else:
                            ins = it[1](e)
                            if it[3]:
                                ins.then_inc(it[2], it[3])
                            else:
                                ins.then_inc(it[2])
                getattr(block, attr)(body)


def _pf_map():
    m = {}
    off = 0
    def add(name, n):
        nonlocal off
        m[name] = (off, n)
        off += n
    add("ident", 128)
    add("cvec", 16)
    add("b_mod", 4 * 48)
    add("norm_g", 4 * 2 * 8)
    add("final_g", 8)
    add("conv_a_w", 2 * 4 * 31)
    add("conv_a_b", 8)
    add("ln_g", 8)
    add("ln_b", 8)
    add("conv_b_w", 2 * 4 * 3)
    add("q_g", 6)
    add("kv_g", 4)
    add("b_pool", 8)
    add("s_pool", 8)
    add("moe_b", 4 * 20)
    add("mask", 128)
    add("pcorr_lat", 4 * 32)
    add("pcorr_ctx", 4 * 32)
    m["_n"] = off
    return m


PFM = _pf_map()
NPF = ((PFM["_n"] + 63) // 64) * 64


def fm(v):
    v = np.asarray(v, np.float32)
    lead = v.shape[:-1]
    n = v.shape[-1] // 128
    v = v.reshape(lead + (n, 128))
    v = np.moveaxis(v, -1, 0)
    return np.ascontiguousarray(v).reshape(128, -1)


def host_tables(core):
    b, r = core // 4, core % 4
    pos = r * NOWN - HALO + np.arange(NLAT)
    valid = (pos >= 0) & (pos < SEQ)
    posc = np.clip(pos, 0, SEQ - 1)
    inv = (10000.0 ** (-np.arange(0, 16, 2, dtype=np.float32) / 16)).astype(np.float32)
    ang = np.concatenate([(posc // 64).astype(np.float32)[:, None] * inv,
                          (posc % 64).astype(np.float32)[:, None] * inv], axis=-1)
    cos, sin = np.cos(ang).astype(np.float32), np.sin(ang).astype(np.float32)
    C = np.zeros((32, NLAT), np.float32)
    S = np.zeros((32, NLAT), np.float32)
    for a in range(2):
        ca, sa = cos[:, 8 * a:8 * a + 8].T, sin[:, 8 * a:8 * a + 8].T
        C[16 * a:16 * a + 8] = ca
        C[16 * a + 8:16 * a + 16] = ca
        S[16 * a:16 * a + 8] = -sa
        S[16 * a + 8:16 * a + 16] = sa
    rope = np.zeros((128, 2, NLAT), np.float32)
    rope[64:96, 0] = C
    rope[64:96, 1] = S
    mask = np.concatenate([valid[:HALO], valid[NLAT - HALO:]]).astype(np.float32)
    mask = np.tile(mask[None, :], (128, 1))

    def corr(posarr, L):
        out = np.ones((4, len(posarr)), np.float32)
        for g, w in enumerate((2, 4, 8, 16)):
            lo = np.clip(posarr - w // 2, 0, L)
            hi = np.clip(posarr - w // 2 + w, 0, L)
            cnt = np.maximum(hi - lo, 1)
            out[g] = w / cnt.astype(np.float32)
        return out
    le = np.concatenate([np.arange(HALO - 8, HALO + 8), np.arange(NLAT - HALO - 8, NLAT - HALO + 8)])
    pc_lat = corr(pos[le], SEQ)
    pc_lat[:, ~valid[le]] = 1.0
    ce = np.concatenate([np.arange(0, 16), np.arange(NCTX - 16, NCTX)])
    pc_ctx = corr(ce, NCTX)
    return rope, mask, np.tile(pc_lat.reshape(1, -1), (128, 1)), np.tile(pc_ctx.reshape(1, -1), (128, 1))


def host_pf(inp, core):
    b = core // 4
    pf = np.zeros((128, NPF), np.float32)
    def put(name, arr):
        o, n = PFM[name]
        arr = np.asarray(arr, np.float32).reshape(128, -1)
        assert arr.shape[1] == n, (name, arr.shape, n)
        pf[:, o:o + n] = arr
    put("ident", np.eye(128, dtype=np.float32))
    cv = np.stack([fm(inp["c"][b]), fm(inp["c_ctx"])], axis=-1)
    put("cvec", cv)
    put("b_mod", fm(inp["b_mod"].reshape(4, 6, 1024)))
    put("norm_g", fm(inp["norm_g"]))
    put("final_g", fm(inp["final_g"]))
    caw = np.transpose(inp["ev_conv_a_w"].reshape(2, 31, 4, 128), (3, 0, 2, 1))
    put("conv_a_w", caw)
    put("conv_a_b", fm(inp["ev_conv_a_b"]))
    put("ln_g", fm(inp["ev_ln_a_g"]))
    put("ln_b", fm(inp["ev_ln_a_b"]))
    put("conv_b_w", np.transpose(inp["ev_conv_b_w"].reshape(2, 3, 4, 128), (3, 0, 2, 1)))
    put("q_g", fm(inp["od_q_norm_g"]))
    put("kv_g", fm(inp["od_kv_norm_g"]))
    put("b_pool", fm(inp["od_b_pool"].reshape(2, 512)))
    put("s_pool", fm(inp["od_s_pool"]))
    mb = np.concatenate([inp["moe_bg"], inp["moe_be"]], axis=-1).reshape(1, 80)
    put("moe_b", np.tile(mb, (128, 1)))
    rope, mask, pcl, pcc = host_tables(core)
    put("mask", mask)
    put("pcorr_lat", pcl)
    put("pcorr_ctx", pcc)
    return pf, rope


def host_weights(inp):
    w = {}
    f = lambda a: np.ascontiguousarray(np.asarray(a, np.float32))
    w["w_mod"] = f(inp["w_mod"])
    w["ev_w_in"] = f(inp["ev_w_in"])
    w["ev_w_out"] = f(inp["ev_w_out"])
    wi = np.asarray(inp["od_w_in"], np.float32)
    q, kv, kr, pool = wi[..., :384], wi[..., 384:640], wi[..., 640:672], wi[..., 672:]
    swap = np.concatenate([np.arange(8, 16), np.arange(0, 8), np.arange(24, 32), np.arange(16, 24)])
    z64 = np.zeros(kr.shape[:-1] + (64,), np.float32)
    w["od_w_in"] = f(np.concatenate([q, kv, pool, z64, kr, z64, kr[..., swap]], axis=-1))
    uq = np.asarray(inp["od_w_uq"], np.float32).reshape(2, 384, 8, 96)
    uqs = np.zeros_like(uq)
    uqs[..., 64:] = uq[..., 64:][..., swap]
    w["od_w_uq"] = f(uq.reshape(2, 384, 768))
    w["od_w_uqs"] = f(uqs.reshape(2, 384, 768))
    w["od_w_ukv"] = f(inp["od_w_ukv"])
    w["od_w_pool"] = f(inp["od_w_pool"])
    w["od_w_out"] = f(inp["od_w_out"])
    w["moe_wge"] = f(np.concatenate([inp["moe_wg"], inp["moe_we"]], axis=-1))
    w["moe_w1"] = f(inp["moe_w1"])
    w["moe_w3"] = f(inp["moe_w3"])
    w["moe_w2"] = f(inp["moe_w2"])
    return w


AW = 52800
OFF_PF = 0
OFF_MOD = OFF_PF + NPF
OFF_ONES = OFF_MOD + 384
OFF_SEL = OFF_ONES + 64
OFF_SELND = OFF_SEL + 1024
OFF_XT = OFF_SELND + 512
OFF_S = OFF_XT + DC * NT
SW = AW - OFF_S


class Builder:
    def __init__(self, layers, in_mode, out_mode):
        self.layers = list(layers)
        self.in_mode = in_mode
        self.out_mode = out_mode
        nc = bass.Bass("TRN2", target_bir_lowering=False)
        self.nc = nc
        self.P = Prog(nc)
        self.arena = nc.alloc_sbuf_tensor("arena", [128, AW], F32).ap()
        self.PS = nc.alloc_psum_tensor("ps", [128, 8, 512], F32).ap()
        self.dram = {}
        self._bank = 0

    def din(self, name, shape, dt=F32):
        if name not in self.dram:
            self.dram[name] = self.nc.dram_tensor(name, list(shape), dt, kind="ExternalInput").ap()
        return self.dram[name]

    def dout(self, name, shape, dt=F32):
        self.dram[name] = self.nc.dram_tensor(name, list(shape), dt, kind="ExternalOutput").ap()
        return self.dram[name]

    def carve(self, off, shape, dt=F32):
        n = int(np.prod(shape[1:]))
        words = n if dt == F32 else (n + 1) // 2
        ap = self.arena[0:shape[0], off:off + words]
        if dt != F32:
            ap = ap.bitcast(dt)
        if len(shape) == 3:
            ap = ap.rearrange("p (a b) -> p a b", a=shape[1])
        elif len(shape) == 4:
            ap = ap.rearrange("p (a b c) -> p a b c", a=shape[1], b=shape[2])
        return ap, off + words

    def pf(self, name):
        o, n = PFM[name]
        return self.arena[:, OFF_PF + o:OFF_PF + o + n]

    def barrier(self):
        P = self.P
        for q in P.streams:
            for k, v in P.semval.items():
                if k == q or v == 0:
                    continue
                if P.known[q].get(k, 0) < v:
                    P.known[q][k] = v
                    P.streams[q].append(("wait", P.sems[k], v))

    def mod(self, l, j, s, c):
        o = OFF_MOD + ((l * 6 + j) * 8 + c) * 2 + s
        return self.arena[:, o:o + 1]

    def prologue(self):
        P, A = self.P, self.arena
        pf_d = self.din("pf", [128, NPF])
        P.dma("sp", A[:, OFF_PF:OFF_PF + NPF], pf_d, w=["PF"])
        self.ONES, _ = self.carve(OFF_ONES, [128, 128], BF16)
        P.op("pool", lambda e: e.memset(self.ONES, 1.0), w=["ONES"])
        self.ID = self.pf("ident")
        self.SEL, _ = self.carve(OFF_SEL, [16, 16, 128], BF16)
        for ex in range(16):
            P.op("dve", lambda e, ex=ex: e.tensor_scalar(out=self.SEL[:, ex, :], in0=self.ONES[0:16, :],
                                                         scalar1=self.ID[0:16, ex:ex + 1], scalar2=None, op0=ALU.mult),
                 r=["PF", "ONES"], w=["SEL"])
        self.SELND, _ = self.carve(OFF_SELND, [65, 4, 128], F32)
        P.op("pool", lambda e: e.memset(self.SELND, 0.0), w=["SELND"])
        P.op("dve", lambda e: e.tensor_copy(out=self.SELND[0:64, 0, 0:64], in_=self.ID[0:64, 0:64]), r=["PF", "SELND"], w=["SELND"])
        P.op("dve", lambda e: e.tensor_copy(out=self.SELND[0:64, 2, 64:128], in_=self.ID[0:64, 0:64]), r=["PF", "SELND"], w=["SELND"])
        P.op("dve", lambda e: e.memset(self.SELND[64:65, 1, 0:64], 1.0), r=["SELND"], w=["SELND"])
        P.op("dve", lambda e: e.memset(self.SELND[64:65, 3, 64:128], 1.0), r=["SELND"], w=["SELND"])
        self.XT, _ = self.carve(OFF_XT, [128, DC, NT], F32)

    def modulation(self):
        P, A, PS = self.P, self.arena, self.PS
        nl = len(self.layers)
        wm = self.din("w_mod", [nl, 1024, 6144])
        off = OFF_S + 2048
        ST, off = self.carve(off, [128, 8, 2], BF16)
        WM = []
        NWM = 6
        for i in range(NWM):
            t, off = self.carve(off, [128, 8, 1024], BF16)
            WM.append(t)
        assert off <= AW
        P.op("act", lambda e: e.activation(out=ST.rearrange("p a b -> p (a b)"), in_=self.pf("cvec"), func=AF.Silu), r=["PF"], w=["ST"])
        n = 0
        for li, l in enumerate(self.layers):
            bank = 4 + li % 2
            for j in range(6):
                slot = n % NWM
                n += 1
                P.dma("pool", WM[slot], wm[li, :, j * 1024:(j + 1) * 1024].rearrange("(c p) m -> p c m", p=128), w=[("WM", slot)])
                for mc in range(8):
                    col = (j * 8 + mc) * 2
                    for k in range(8):
                        P.op("pe", lambda e, slot=slot, mc=mc, k=k, col=col, bank=bank: e.matmul(
                            PS[:, bank, col:col + 2], lhsT=WM[slot][:, k, mc * 128:(mc + 1) * 128], rhs=ST[:, k, :],
                            start=(k == 0), stop=(k == 7)), r=[("WM", slot), "ST"], w=[("PS", bank)])
            o, _ = PFM["b_mod"]
            BM = A[:, OFF_PF + o + l * 48:OFF_PF + o + (l + 1) * 48]
            MODL = A[:, OFF_MOD + li * 96:OFF_MOD + (li + 1) * 96].rearrange("p (a s) -> p a s", s=2)
            PSV = PS[:, bank, 0:96].rearrange("p (a s) -> p a s", s=2)
            for s in range(2):
                P.op("dve", lambda e, s=s, MODL=MODL, PSV=PSV, BM=BM: e.tensor_tensor(out=MODL[:, :, s], in0=PSV[:, :, s], in1=BM, op=ALU.add),
                     r=[("PS", bank), "PF"], w=["MOD"])
            o, _ = PFM["norm_g"]
            for (j, gi) in ((1, 0), (4, 1)):
                G = A[:, OFF_PF + o + (l * 2 + gi) * 8:OFF_PF + o + (l * 2 + gi) * 8 + 8]
                for s in range(2):
                    V = MODL[:, j * 8:(j + 1) * 8, s]
                    P.op("dve", lambda e, V=V, G=G: e.scalar_tensor_tensor(out=V, in0=V, scalar=1.0, in1=G, op0=ALU.add, op1=ALU.mult),
                         r=["MOD", "PF"], w=["MOD"])
        self.barrier()

    def load_x_raw(self):
        P, PS, XT = self.P, self.PS, self.XT
        x_in = self.din("x_in", [NLAT, 1024])
        c_in = self.din("ctx_in", [NCTX, 1024])
        off = OFF_S
        XS = []
        for i in range(2):
            t, off = self.carve(off, [128, 1024], F32)
            XS.append(t)
        for t in range(NT // 128):
            slot = t % 2
            src = x_in[t * 128:(t + 1) * 128, :] if t < NLAT // 128 else c_in[(t - NLAT // 128) * 128:(t - NLAT // 128 + 1) * 128, :]
            P.dma("sp", XS[slot], src, w=[("XS", slot)])
            for half in range(2):
                bank = 2 * slot + half
                for c4 in range(4):
                    c = half * 4 + c4
                    P.op("pe", lambda e, slot=slot, c=c, c4=c4, bank=bank: e.transpose(
                        out=PS[:, bank, c4 * 128:(c4 + 1) * 128], in_=XS[slot][:, c * 128:(c + 1) * 128], identity=self.ID),
                        r=[("XS", slot), "PF"], w=[("PS", bank)])
                dst = XT[:, half * 4:half * 4 + 4, t * 128:(t + 1) * 128]
                srcp = PS[:, bank, :].rearrange("p (a b) -> p a b", a=4)
                if half == 0:
                    P.op("act", lambda e, dst=dst, srcp=srcp: e.activation(out=dst, in_=srcp, func=AF.Copy), r=[("PS", bank)], w=[("XT", t // 4 if t < 16 else 4)])
                else:
                    P.op("dve", lambda e, dst=dst, srcp=srcp: e.tensor_copy(out=dst, in_=srcp), r=[("PS", bank)], w=[("XT", t // 4 if t < 16 else 4)])

    def load_x_state(self):
        st = self.din("st_in", [128, DC * NT])
        for b, (c0, c1) in enumerate(BLOCKS):
            self.P.dma("sp", self.XT[:, :, c0:c1], st.rearrange("p (a b) -> p a b", a=DC)[:, :, c0:c1], w=[("XT", b)])
        self.barrier()

    def store_x_state(self):
        self.barrier()
        st = self.dout("st_out", [128, DC * NT])
        for b, (c0, c1) in enumerate(BLOCKS):
            self.P.dma("sp", st.rearrange("p (a b) -> p a b", a=DC)[:, :, c0:c1], self.XT[:, :, c0:c1], r=[("XT", b)], w=["OUT"])
        self.P.wait_all("sp", ["OUT"])
        self.barrier()

    def norm_scratch(self, off):
        self.nSQ, off = self.carve(off, [128, DC, 512], BF16)
        self.nRS, off = self.carve(off, [128, 512], F32)
        self.nTM = []
        for i in range(2):
            t, off = self.carve(off, [128, 512], F32)
            self.nTM.append(t)
        self._ntm = 0
        return off

    def norm_mod(self, b, dst_fn, lslot, jA, jB, wres, rng=None, psum_rs=False):
        P, PS, XT = self.P, self.PS, self.XT
        c0, c1 = rng if rng is not None else BLOCKS[b]
        nb = c1 - c0
        SQ = self.nSQ
        if psum_rs:
            self._nrs = getattr(self, "_nrs", 0) + 1
            ri = self._nrs % 2
            RS = PS[:, ri, :]
            rsr = ("PS", ri)
        else:
            RS = self.nRS
            rsr = "nRS"
        P.op("act", lambda e: e.activation(out=SQ[:, :, :nb], in_=XT[:, :, c0:c1], func=AF.Square), r=[("XT", b)], w=["nSQ"])
        for c in range(DC):
            P.op("pe", lambda e, c=c: e.matmul(PS[:, 7, :nb], lhsT=self.ONES, rhs=SQ[:, c, :nb], start=(c == 0), stop=(c == DC - 1)),
                 r=["nSQ", "ONES"], w=[("PS", 7)])
        P.op("dve", lambda e: e.tensor_scalar(out=RS[:, :nb], in0=PS[:, 7, :nb], scalar1=1.0 / D, scalar2=EPS, op0=ALU.mult, op1=ALU.add),
             r=[("PS", 7)], w=[rsr])
        P.op("act", lambda e: e.activation(out=RS[:, :nb], in_=RS[:, :nb], func=AF.Sqrt), r=[rsr], w=[rsr])
        P.op("dve", lambda e: e.reciprocal(out=RS[:, :nb], in_=RS[:, :nb]), r=[rsr], w=[rsr])
        for c in range(DC):
            for (s0, s1, s) in segs(c0, c1):
                i = self._ntm % 2
                self._ntm += 1
                TM = self.nTM[i]
                n = s1 - s0
                P.op("dve", lambda e, c=c, s0=s0, s1=s1, s=s, TM=TM, n=n: e.scalar_tensor_tensor(
                    out=TM[:, :n], in0=XT[:, c, s0:s1], scalar=self.mod(lslot, jA, s, c), in1=RS[:, s0 - c0:s1 - c0],
                    op0=ALU.mult, op1=ALU.mult), r=[("XT", b), rsr, "MOD"], w=[("nTM", i)])
                dst = dst_fn(c, s0, s1)
                P.op("act", lambda e, dst=dst, TM=TM, n=n, c=c, s=s: e.activation(
                    out=dst, in_=TM[:, :n], func=AF.Identity, bias=self.mod(lslot, jB, s, c), scale=1.0),
                    r=[("nTM", i), "MOD"], w=wres)

    def even_mixer(self, lslot, l):
        P, PS, XT, A = self.P, self.PS, self.XT, self.arena
        j = l // 2
        nev = len([x for x in self.layers if x % 2 == 0])
        jslot = [x for x in self.layers if x % 2 == 0].index(l)
        w_in = self.din("ev_w_in", [nev, 1024, 2560])
        w_out = self.din("ev_w_out", [nev, 1024, 1024])
        off = OFF_S
        HL, off = self.carve(off, [128, DC, NT], BF16)
        APAD, off = self.carve(off, [128, 4, NPAD], BF16)
        GCH, off = self.carve(off, [128, 4, NPAD], BF16)
        GB, off = self.carve(off, [128, 4, NT], BF16)
        offW = off
        WP = []
        for i in range(2):
            t, off = self.carve(off, [128, DC, 512], BF16)
            WP.append(t)
        SG = []
        for i in range(2):
            t, off = self.carve(off, [128, 512], F32)
            SG.append(t)
        assert off <= AW, off
        self.norm_scratch(OFF_S + DC * NT // 2 + 4 * NPAD // 2)
        P.op("pool", lambda e: e.memset(APAD, 0.0), w=["APAD"])
        P.dma("pool", WP[0], w_in[jslot, :, 0:512].rearrange("(c p) m -> p c m", p=128), w=[("WP", 0)])
        P.dma("pool", WP[1], w_in[jslot, :, 512:1024].rearrange("(c p) m -> p c m", p=128), w=[("WP", 1)])
        for b in range(5):
            self.norm_mod(b, lambda c, s0, s1: HL[:, c, s0:s1], lslot, 1, 0, [("HL", b)], psum_rs=True)
        self.barrier()
        P.op("pool", lambda e: e.memset(GCH, 0.0), w=["GCH"])
        MASK = self.pf("mask")
        def load_piece(slot, pc):
            P.dma("pool", WP[slot], w_in[jslot, :, pc * 512:(pc + 1) * 512].rearrange("(c p) m -> p c m", p=128), w=[("WP", slot)])
        def mm_piece(slot, b, mc, bank):
            c0, c1 = BLOCKS[b]
            for k in range(DC):
                P.op("pe", lambda e, k=k: e.matmul(PS[:, bank, :c1 - c0], lhsT=WP[slot][:, k, mc * 128:(mc + 1) * 128], rhs=HL[:, k, c0:c1],
                                                   start=(k == 0), stop=(k == DC - 1)), r=[("WP", slot), ("HL", b)], w=[("PS", bank)])
        for (p0, p1, DST, dres) in ((0, 1, APAD, "APAD"), (3, 4, GCH, "GCH")):
            if p0 != 0:
                load_piece(0, p0)
                load_piece(1, p1)
            n = 0
            for b in range(5):
                c0, c1 = BLOCKS[b]
                nb = c1 - c0
                for mc in range(4):
                    b0, b1 = (n % 2) * 2, (n % 2) * 2 + 1
                    sg = SG[n % 2]
                    n += 1
                    mm_piece(0, b, mc, b0)
                    mm_piece(1, b, mc, b1)
                    if DST is APAD:
                        P.op("act", lambda e, sg=sg, b1=b1, nb=nb: e.activation(out=sg[:, :nb], in_=PS[:, b1, :nb], func=AF.Sigmoid),
                             r=[("PS", b1)], w=[("SG", n % 2)])
                    else:
                        P.op("act", lambda e, sg=sg, b1=b1, nb=nb: e.activation(out=sg[:, :nb], in_=PS[:, b1, :nb], func=AF.Copy),
                             r=[("PS", b1)], w=[("SG", n % 2)])
                    for (s0, s1, s) in segs(c0, c1):
                        P.op("dve", lambda e, sg=sg, b0=b0, s0=s0, s1=s1, mc=mc, DST=DST: e.tensor_tensor(
                            out=DST[:, mc, pcol(s0):pcol(s0) + s1 - s0], in0=PS[:, b0, s0 - c0:s1 - c0], in1=sg[:, s0 - c0:s1 - c0], op=ALU.mult),
                            r=[("PS", b0), ("SG", n % 2)], w=[dres])
            for mc in range(4):
                for (pc0, m0) in ((PADW, 0), (PADW + NLAT - HALO, 64)):
                    P.op("pool", lambda e, mc=mc, pc0=pc0, m0=m0, DST=DST: e.tensor_tensor(
                        out=DST[:, mc, pc0:pc0 + HALO], in0=DST[:, mc, pc0:pc0 + HALO], in1=MASK[:, m0:m0 + HALO], op=ALU.mult),
                        r=[dres, "PF"], w=[dres])
        load_piece(0, 2)
        n = 0
        for b in range(5):
            c0, c1 = BLOCKS[b]
            nb = c1 - c0
            for mc in range(4):
                bank = n % 4
                n += 1
                mm_piece(0, b, mc, bank)
                P.op("act", lambda e, bank=bank, mc=mc, c0=c0, c1=c1, nb=nb: e.activation(out=GB[:, mc, c0:c1], in_=PS[:, bank, :nb], func=AF.Copy),
                     r=[("PS", bank)], w=["GB"])
        self.barrier()
        off = OFF_S
        AOUT, off = self.carve(off, [128, 4, NT], BF16)
        DIAG, off = self.carve(off, [128, 31, 128], BF16)
        IDB, off = self.carve(off, [128, 128], BF16)
        SQ2, off = self.carve(off, [128, 4, 512], BF16)
        MEAN, off = self.carve(off, [128, 512], F32)
        VAR, off = self.carve(off, [128, 512], F32)
        TA, off = self.carve(off, [128, 512], F32)
        assert off <= OFF_S + DC * NT // 2, (off, OFF_S + DC * NT // 2)
        off = offW
        WO, off = self.carve(off, [128, DC, 1024], BF16)
        C3a, off = self.carve(off, [128, 512], F32)
        C3b, off = self.carve(off, [128, 512], F32)
        assert off <= AW
        P.dma("pool", WO, w_out[jslot].rearrange("(c p) m -> p c m", p=128), w=["WO"])
        P.op("dve", lambda e: e.tensor_copy(out=IDB, in_=self.ID), r=["PF"], w=["IDB"])
        o, _ = PFM["conv_a_w"]
        CAW = A[:, OFF_PF + o + j * 124:OFF_PF + o + (j + 1) * 124].rearrange("p (a b) -> p a b", a=4)
        o, _ = PFM["conv_a_b"]
        CAB = A[:, OFF_PF + o + j * 4:OFF_PF + o + (j + 1) * 4]
        o, _ = PFM["ln_g"]
        LG = A[:, OFF_PF + o + j * 4:OFF_PF + o + (j + 1) * 4]
        o, _ = PFM["ln_b"]
        LB = A[:, OFF_PF + o + j * 4:OFF_PF + o + (j + 1) * 4]
        o, _ = PFM["conv_b_w"]
        CBW = A[:, OFF_PF + o + j * 12:OFF_PF + o + (j + 1) * 12].rearrange("p (a b) -> p a b", a=4)
        cblocks = [(PADW + i * 512, 512, i * 512) for i in range(4)] + [(PADW + 2048, 128, 2048), (pcol(NLAT), NCTX, NLAT)]
        nb_ = 0
        for mc in range(4):
            for k in range(31):
                P.op("act", lambda e, k=k: e.activation(out=DIAG[:, k, :], in_=IDB, func=AF.Copy, scale=CAW[:, mc, k:k + 1]),
                     r=["IDB", "PF"], w=["DIAG"])
            for (p0, n, q0) in cblocks:
                bank = nb_ % 4
                nb_ += 1
                for k in range(31):
                    P.op("pe", lambda e, k=k: e.matmul(PS[:, bank, :n], lhsT=DIAG[:, k, :], rhs=APAD[:, mc, p0 - 15 + k:p0 - 15 + k + n],
                                                      start=(k == 0), stop=(k == 30)), r=["DIAG", "APAD"], w=[("PS", bank)])
                P.op("dve", lambda e: e.tensor_scalar(out=AOUT[:, mc, q0:q0 + n], in0=PS[:, bank, :n], scalar1=CAB[:, mc:mc + 1], scalar2=None, op0=ALU.add),
                     r=[("PS", bank), "PF"], w=[("AOUT", q0)])
        for (p0, n, q0) in cblocks:
            ar = [("AOUT", q0)]
            P.op("act", lambda e: e.activation(out=SQ2[:, :, :n], in_=AOUT[:, :, q0:q0 + n], func=AF.Square), r=ar, w=["SQ2"])
            for mc in range(4):
                P.op("pe", lambda e, mc=mc: e.matmul(PS[:, 4, :n], lhsT=self.ONES, rhs=AOUT[:, mc, q0:q0 + n], start=(mc == 0), stop=(mc == 3)),
                     r=ar + ["ONES"], w=[("PS", 4)])
            for mc in range(4):
                P.op("pe", lambda e, mc=mc: e.matmul(PS[:, 5, :n], lhsT=self.ONES, rhs=SQ2[:, mc, :n], start=(mc == 0), stop=(mc == 3)),
                     r=["SQ2", "ONES"], w=[("PS", 5)])
            P.op("act", lambda e: e.activation(out=MEAN[:, :n], in_=PS[:, 4, :n], func=AF.Copy, scale=1.0 / 512), r=[("PS", 4)], w=["MEAN"])
            P.op("dve", lambda e: e.tensor_tensor(out=TA[:, :n], in0=MEAN[:, :n], in1=MEAN[:, :n], op=ALU.mult), r=["MEAN"], w=["TA"])
            P.op("dve", lambda e: e.scalar_tensor_tensor(out=VAR[:, :n], in0=PS[:, 5, :n], scalar=1.0 / 512, in1=TA[:, :n],
                                                         op0=ALU.mult, op1=ALU.subtract), r=[("PS", 5), "TA"], w=["VAR"])
            P.op("dve", lambda e: e.tensor_scalar(out=VAR[:, :n], in0=VAR[:, :n], scalar1=EPS, scalar2=None, op0=ALU.add), r=["VAR"], w=["VAR"])
            P.op("act", lambda e: e.activation(out=VAR[:, :n], in_=VAR[:, :n], func=AF.Sqrt), r=["VAR"], w=["VAR"])
            P.op("dve", lambda e: e.reciprocal(out=VAR[:, :n], in_=VAR[:, :n]), r=["VAR"], w=["VAR"])
            for mc in range(4):
                P.op("dve", lambda e, mc=mc: e.tensor_tensor(out=TA[:, :n], in0=AOUT[:, mc, q0:q0 + n], in1=MEAN[:, :n], op=ALU.subtract),
                     r=ar + ["MEAN"], w=["TA"])
                P.op("dve", lambda e, mc=mc: e.tensor_tensor(out=TA[:, :n], in0=TA[:, :n], in1=VAR[:, :n], op=ALU.mult),
                     r=["TA", "VAR"], w=["TA"])
                P.op("act", lambda e, mc=mc: e.activation(out=AOUT[:, mc, q0:q0 + n], in_=TA[:, :n], func=AF.Silu,
                                                         bias=LB[:, mc:mc + 1], scale=LG[:, mc:mc + 1]), r=["TA", "PF"], w=ar)
            for mc in range(4):
                P.op("dve", lambda e, mc=mc: e.tensor_scalar(out=C3a[:, :n], in0=GCH[:, mc, p0 - 1:p0 - 1 + n], scalar1=CBW[:, mc, 0:1], scalar2=None, op0=ALU.mult),
                     r=["GCH", "PF"], w=["C3a"])
                for k in (1, 2):
                    P.op("dve", lambda e, mc=mc, k=k: e.scalar_tensor_tensor(out=C3a[:, :n], in0=GCH[:, mc, p0 - 1 + k:p0 - 1 + k + n], scalar=CBW[:, mc, k:k + 1],
                                                                         in1=C3a[:, :n], op0=ALU.mult, op1=ALU.add), r=["GCH", "PF", "C3a"], w=["C3a"])
                P.op("pool", lambda e, mc=mc: e.tensor_tensor(out=GB[:, mc, q0:q0 + n], in0=GB[:, mc, q0:q0 + n], in1=C3a[:, :n], op=ALU.mult),
                     r=["C3a", "GB"], w=["GB"])
        self.barrier()
        n = 0
        for b in range(5):
            c0, c1 = BLOCKS[b]
            nb = c1 - c0
            for mc in range(DC):
                bank = n % 4
                n += 1
                for k in range(DC):
                    src = AOUT[:, k, c0:c1] if k < 4 else GB[:, k - 4, c0:c1]
                    P.op("pe", lambda e, k=k, src=src, mc=mc, bank=bank, nb=nb: e.matmul(
                        PS[:, bank, :nb], lhsT=WO[:, k, mc * 128:(mc + 1) * 128], rhs=src, start=(k == 0), stop=(k == DC - 1)),
                        r=["WO", "GB"] + [("AOUT", q) for q in (0, 512, 1024, 1536, 2048, NLAT)], w=[("PS", bank)])
                for (s0, s1, s) in segs(c0, c1):
                    P.op("dve", lambda e, mc=mc, bank=bank, s0=s0, s1=s1, s=s, c0=c0: e.scalar_tensor_tensor(
                        out=XT[:, mc, s0:s1], in0=PS[:, bank, s0 - c0:s1 - c0], scalar=self.mod(lslot, 2, s, mc), in1=XT[:, mc, s0:s1],
                        op0=ALU.mult, op1=ALU.add), r=[("PS", bank), ("XT", b), "MOD"], w=[("XT", b)])
        self.barrier()

    def moe(self, lslot, l):
        P, PS, XT, A = self.P, self.PS, self.XT, self.arena
        last = (l == 3)
        MB_ = [(HALO + i * 512, HALO + (i + 1) * 512) for i in range(4)] if last else BLOCKS
        NB_ = len(MB_)
        nl = len(self.layers)
        wge = self.din("moe_wge", [nl, 1024, 20])
        w1 = self.din("moe_w1", [nl, 16, 1024, 512])
        w3 = self.din("moe_w3", [nl, 16, 1024, 512])
        w2 = self.din("moe_w2", [nl, 16, 512, 1024])
        off = OFF_S
        T, off = self.carve(off, [128, DC, NT], BF16)
        W1, W3, W2 = [], [], []
        for i in range(2):
            t, off = self.carve(off, [128, DC, 512], BF16); W1.append(t)
            t, off = self.carve(off, [128, DC, 512], BF16); W3.append(t)
            t, off = self.carve(off, [128, 4, 1024], BF16); W2.append(t)
        GTH, off = self.carve(off, [16, NT], BF16)
        GTL, off = self.carve(off, [16, NT], BF16)
        WGE, off = self.carve(off, [128, DC, 20], BF16)
        RT, off = self.carve(off, [128, 176], F32)
        offX = off
        self.norm_scratch(offX)
        def load_expert(ex):
            sl = ex % 2
            P.dma("pool", W1[sl], w1[lslot, ex].rearrange("(c p) m -> p c m", p=128), w=[("W1", sl)])
            P.dma("pool", W3[sl], w3[lslot, ex].rearrange("(c p) m -> p c m", p=128), w=[("W3", sl)])
            P.dma("pool", W2[sl], w2[lslot, ex].rearrange("(c p) m -> p c m", p=128), w=[("W2", sl)])
        load_expert(0)
        load_expert(1)
        for b in range(NB_):
            self.norm_mod(b, lambda c, s0, s1: T[:, c, s0:s1], lslot, 4, 3, [("T", b)], rng=MB_[b], psum_rs=True)
        P.dma("pool", WGE, wge[lslot].rearrange("(c p) m -> p c m", p=128), w=["WGE"])
        self.barrier()
        o, _ = PFM["moe_b"]
        MB = A[:, OFF_PF + o + l * 20:OFF_PF + o + (l + 1) * 20]
        NTL = 16 if last else NT // 128
        tbase = HALO if last else 0
        ro = offX
        LGr, ro = self.carve(ro, [128, NTL, 20], F32)
        GOHr, ro = self.carve(ro, [128, NTL, 4], F32)
        GEXr, ro = self.carve(ro, [128, NTL, 4], F32)
        EMr, ro = self.carve(ro, [128, NTL, 16], F32)
        OH1r, ro = self.carve(ro, [128, NTL, 16], F32)
        OH2r, ro = self.carve(ro, [128, NTL, 16], F32)
        GAr, ro = self.carve(ro, [128, NTL, 16], F32)
        GHFr, ro = self.carve(ro, [128, NTL, 16], F32)
        GLOr, ro = self.carve(ro, [128, NTL, 16], F32)
        GHBr, ro = self.carve(ro, [128, NTL, 16], BF16)
        SCr, ro = self.carve(ro, [128, 8, NTL], F32)
        assert ro <= AW
        GMv, GWv, M1v, M2v, E2v, W1v = SCr[:, 0, :], SCr[:, 1, :], SCr[:, 2, :], SCr[:, 3, :], SCr[:, 4, :], SCr[:, 5, :]
        BIG = 1.0e30
        X_ = mybir.AxisListType.X
        rt = ["RT"]
        for t in range(NTL):
            b = min(t // 4, 4)
            tc0 = tbase + t * 128
            for c in range(DC):
                P.op("pe", lambda e, c=c: e.matmul(PS[:, 6, t * 20:(t + 1) * 20], lhsT=T[:, c, tc0:tc0 + 128], rhs=WGE[:, c, :],
                                                  start=(c == 0), stop=(c == DC - 1)), r=[("T", b), "WGE"], w=[("PS", 6)])
        PSV = PS[:, 6, 0:NTL * 20].rearrange("p (t k) -> p t k", k=20)
        for jj in range(20):
            P.op("dve", lambda e, jj=jj: e.tensor_scalar(out=LGr[:, :, jj], in0=PSV[:, :, jj], scalar1=MB[:, jj:jj + 1], scalar2=None, op0=ALU.add),
                 r=[("PS", 6), "PF"], w=rt)
        P.op("dve", lambda e: e.reduce_max(out=GMv, in_=LGr[:, :, 0:4], axis=X_), r=rt, w=rt)
        for g in range(4):
            P.op("dve", lambda e, g=g: e.tensor_tensor(out=GOHr[:, :, g], in0=LGr[:, :, g], in1=GMv, op=ALU.is_ge), r=rt, w=rt)
            P.op("dve", lambda e, g=g: e.tensor_tensor(out=GEXr[:, :, g], in0=LGr[:, :, g], in1=GMv, op=ALU.subtract), r=rt, w=rt)
        P.op("act", lambda e: e.activation(out=GEXr, in_=GEXr, func=AF.Exp), r=rt, w=rt)
        P.op("dve", lambda e: e.reduce_sum(out=GWv, in_=GEXr, axis=X_), r=rt, w=rt)
        P.op("dve", lambda e: e.reciprocal(out=GWv, in_=GWv), r=rt, w=rt)
        P.op("dve", lambda e: e.tensor_scalar(out=GOHr, in0=GOHr, scalar1=-1.0, scalar2=BIG, op0=ALU.add, op1=ALU.mult), r=rt, w=rt)
        for g in range(4):
            for i in range(4):
                jj = g * 4 + i
                P.op("dve", lambda e, g=g, jj=jj: e.tensor_tensor(out=EMr[:, :, jj], in0=LGr[:, :, 4 + jj], in1=GOHr[:, :, g], op=ALU.add), r=rt, w=rt)
        P.op("dve", lambda e: e.reduce_max(out=M1v, in_=EMr, axis=X_), r=rt, w=rt)
        for jj in range(16):
            P.op("dve", lambda e, jj=jj: e.tensor_tensor(out=OH1r[:, :, jj], in0=EMr[:, :, jj], in1=M1v, op=ALU.is_ge), r=rt, w=rt)
        P.op("dve", lambda e: e.scalar_tensor_tensor(out=EMr, in0=OH1r, scalar=-BIG, in1=EMr, op0=ALU.mult, op1=ALU.add), r=rt, w=rt)
        P.op("dve", lambda e: e.reduce_max(out=M2v, in_=EMr, axis=X_), r=rt, w=rt)
        for jj in range(16):
            P.op("dve", lambda e, jj=jj: e.tensor_tensor(out=OH2r[:, :, jj], in0=EMr[:, :, jj], in1=M2v, op=ALU.is_ge), r=rt, w=rt)
        P.op("dve", lambda e: e.tensor_tensor(out=E2v, in0=M2v, in1=M1v, op=ALU.subtract), r=rt, w=rt)
        P.op("act", lambda e: e.activation(out=E2v, in_=E2v, func=AF.Exp), r=rt, w=rt)
        P.op("dve", lambda e: e.tensor_scalar(out=W1v, in0=E2v, scalar1=1.0, scalar2=None, op0=ALU.add), r=rt, w=rt)
        P.op("dve", lambda e: e.reciprocal(out=W1v, in_=W1v), r=rt, w=rt)
        P.op("dve", lambda e: e.tensor_tensor(out=E2v, in0=E2v, in1=W1v, op=ALU.mult), r=rt, w=rt)
        P.op("dve", lambda e: e.tensor_tensor(out=W1v, in0=W1v, in1=GWv, op=ALU.mult), r=rt, w=rt)
        P.op("dve", lambda e: e.tensor_tensor(out=E2v, in0=E2v, in1=GWv, op=ALU.mult), r=rt, w=rt)
        for jj in range(16):
            P.op("dve", lambda e, jj=jj: e.tensor_tensor(out=GAr[:, :, jj], in0=OH1r[:, :, jj], in1=W1v, op=ALU.mult), r=rt, w=rt)
            P.op("dve", lambda e, jj=jj: e.tensor_tensor(out=OH2r[:, :, jj], in0=OH2r[:, :, jj], in1=E2v, op=ALU.mult), r=rt, w=rt)
        P.op("dve", lambda e: e.tensor_tensor(out=GAr, in0=GAr, in1=OH2r, op=ALU.add), r=rt, w=rt)
        P.op("dve", lambda e: e.tensor_copy(out=GHBr, in_=GAr), r=rt, w=rt)
        P.op("dve", lambda e: e.tensor_copy(out=GHFr, in_=GHBr), r=rt, w=rt)
        P.op("dve", lambda e: e.tensor_tensor(out=GLOr, in0=GAr, in1=GHFr, op=ALU.subtract), r=rt, w=rt)
        for t0_ in range(0, NTL, 4):
            nt_ = min(4, NTL - t0_)
            for tt in range(nt_):
                t = t0_ + tt
                P.op("pe", lambda e, t=t, tt=tt: e.transpose(out=PS[0:16, 5, tt * 128:(tt + 1) * 128], in_=GHFr[:, t, :], identity=self.ID), r=rt + ["PF"], w=[("PS", 5)])
                P.op("pe", lambda e, t=t, tt=tt: e.transpose(out=PS[0:16, 4, tt * 128:(tt + 1) * 128], in_=GLOr[:, t, :], identity=self.ID), r=rt + ["PF"], w=[("PS", 4)])
            c0g = tbase + t0_ * 128
            gres = [("GT", bb) for bb in range(NB_)]
            P.op("act", lambda e: e.activation(out=GTH[:, c0g:c0g + nt_ * 128], in_=PS[0:16, 5, 0:nt_ * 128], func=AF.Copy), r=[("PS", 5)], w=gres)
            P.op("act", lambda e: e.activation(out=GTL[:, c0g:c0g + nt_ * 128], in_=PS[0:16, 4, 0:nt_ * 128], func=AF.Copy), r=[("PS", 4)], w=gres)
        self.barrier()
        off = offX
        HID, SGs, HGs, GBC = [], [], [], []
        for i in range(2):
            t, off = self.carve(off, [128, 4, 512], BF16); HID.append(t)
        for i in range(2):
            t, off = self.carve(off, [128, 512], F32); SGs.append(t)
            t, off = self.carve(off, [128, 512], F32); HGs.append(t)
        for i in range(2):
            t, off = self.carve(off, [128, 512], F32); GBC.append(t)
        assert off <= AW, (off, AW)
        nh = 0
        no = 0
        def h_phase(ex, b, hb):
            nonlocal nh
            sl = ex % 2
            c0, c1 = MB_[b]
            nb = c1 - c0
            gbc = GBC[hb]
            P.op("pe", lambda e: e.matmul(PS[:, 6, :nb], lhsT=self.SEL[:, ex, :], rhs=GTH[:, c0:c1], start=True, stop=False),
                 r=["SEL", ("GT", b)], w=[("PS", 6)])
            P.op("pe", lambda e: e.matmul(PS[:, 6, :nb], lhsT=self.SEL[:, ex, :], rhs=GTL[:, c0:c1], start=False, stop=True),
                 r=["SEL", ("GT", b)], w=[("PS", 6)])
            P.op("act", lambda e: e.activation(out=gbc[:, :nb], in_=PS[:, 6, :nb], func=AF.Copy), r=[("PS", 6)], w=[("GBC", hb)])
            for hc in range(4):
                i2 = nh % 2
                nh += 1
                b1, b3 = i2, 2 + i2
                for k in range(DC):
                    P.op("pe", lambda e, k=k: e.matmul(PS[:, b1, :nb], lhsT=W1[sl][:, k, hc * 128:(hc + 1) * 128], rhs=T[:, k, c0:c1],
                                                      start=(k == 0), stop=(k == DC - 1)), r=[("W1", sl), ("T", b)], w=[("PS", b1)])
                for k in range(DC):
                    P.op("pe", lambda e, k=k: e.matmul(PS[:, b3, :nb], lhsT=W3[sl][:, k, hc * 128:(hc + 1) * 128], rhs=T[:, k, c0:c1],
                                                      start=(k == 0), stop=(k == DC - 1)), r=[("W3", sl), ("T", b)], w=[("PS", b3)])
                sg, hg = SGs[i2], HGs[i2]
                P.op("act", lambda e: e.activation(out=sg[:, :nb], in_=PS[:, b1, :nb], func=AF.Silu), r=[("PS", b1)], w=[("SGs", i2)])
                P.op("dve", lambda e: e.tensor_tensor(out=hg[:, :nb], in0=sg[:, :nb], in1=PS[:, b3, :nb], op=ALU.mult),
                     r=[("SGs", i2), ("PS", b3)], w=[("HGs", i2)])
                P.op("pool", lambda e: e.tensor_tensor(out=HID[hb][:, hc, :nb], in0=hg[:, :nb], in1=gbc[:, :nb], op=ALU.mult),
                     r=[("HGs", i2), ("GBC", hb)], w=[("HID", hb)])

        def w2_phase(ex, b, hb):
            nonlocal no
            sl = ex % 2
            c0, c1 = MB_[b]
            nb = c1 - c0
            for fc in range(DC):
                bo = 4 + no % 2
                no += 1
                for hc in range(4):
                    P.op("pe", lambda e, hc=hc: e.matmul(PS[:, bo, :nb], lhsT=W2[sl][:, hc, fc * 128:(fc + 1) * 128], rhs=HID[hb][:, hc, :nb],
                                                        start=(hc == 0), stop=(hc == 3)), r=[("W2", sl), ("HID", hb)], w=[("PS", bo)])
                for (s0, s1, s) in segs(c0, c1):
                    P.op("dve", lambda e: e.scalar_tensor_tensor(
                        out=XT[:, fc, s0:s1], in0=PS[:, bo, s0 - c0:s1 - c0], scalar=self.mod(lslot, 5, s, fc), in1=XT[:, fc, s0:s1],
                        op0=ALU.mult, op1=ALU.add), r=[("PS", bo), ("XT", b), "MOD"], w=[("XT", b)])

        its = [(ex, b) for ex in range(16) for b in range(NB_)]
        prev = None
        for n_it, (ex, b) in enumerate(its):
            hb = n_it % 2
            h_phase(ex, b, hb)
            if prev is not None:
                w2_phase(*prev)
            prev = (ex, b, hb)
            if b == NB_ - 1 and ex + 2 < 16:
                pending = ex + 2
            if b == 0 and ex >= 1 and ex + 1 < 16:
                load_expert(ex + 1)
        w2_phase(*prev)
        self.barrier()

    def odd_mixer(self, lslot, l):
        P, PS, XT, A, nc = self.P, self.PS, self.XT, self.arena, self.nc
        j = l // 2
        ods = [x for x in self.layers if x % 2 == 1]
        nod = len(ods)
        js = ods.index(l)
        ctx_out = (l != 3)
        w_in = self.din("od_w_in", [nod, 1024, 1344])
        w_uq = self.din("od_w_uq", [nod, 384, 768])
        w_uqs = self.din("od_w_uqs", [nod, 384, 768])
        w_ukv = self.din("od_w_ukv", [nod, 256, 1024])
        w_pool = self.din("od_w_pool", [nod, 4, 128, 128])
        w_out = self.din("od_w_out", [nod, 1024, 1024])
        rope_d = self.din("rope", [128, 2, NLAT])
        XSP = nc.dram_tensor("xsp%d" % l, [128, DC * NT], F32).ap().rearrange("p (a b) -> p a b", a=DC)
        XI = nc.dram_tensor("xi%d" % l, [256, NOWN], BF16)
        XO = nc.dram_tensor("xo%d" % l, [4 * 256, NOWN], BF16)
        XI2 = nc.dram_tensor("xj%d" % l, [256, NOWN], BF16)
        XO2 = nc.dram_tensor("xp%d" % l, [4 * 256, NOWN], BF16)
        cck = "cc%d" % l
        P.sems[cck] = nc.alloc_semaphore(cck)
        P.semval[cck] = 0
        cck2 = "cd%d" % l
        P.sems[cck2] = nc.alloc_semaphore(cck2)
        P.semval[cck2] = 0
        off = OFF_S
        QN, off = self.carve(off, [128, 3, NT], BF16)
        KVN, off = self.carve(off, [128, 2, NT], BF16)
        KRT, off = self.carve(off, [128, NT], BF16)
        ROPE, off = self.carve(off, [128, 2, NLAT], F32)
        UP, off = self.carve(off, [128, 4, NPAD], BF16)
        offL = off
        HLB, off = self.carve(off, [128, DC, 512], BF16)
        WIN, off = self.carve(off, [128, DC, 1344], BF16)
        off = self.norm_scratch(off)
        QC, off = self.carve(off, [128, 3, 512], F32)
        assert off <= AW, (off, AW)
        MASK = self.pf("mask")
        o, _ = PFM["q_g"]
        QG = A[:, OFF_PF + o + j * 3:OFF_PF + o + j * 3 + 3]
        o, _ = PFM["kv_g"]
        KG = A[:, OFF_PF + o + j * 2:OFF_PF + o + j * 2 + 2]
        SQ, RS = self.nSQ, self.nRS
        rot = [0]
        def nbank(lo=0, n=6):
            rot[0] += 1
            return lo + rot[0] % n
        P.dma("pool", WIN, w_in[js].rearrange("(c p) m -> p c m", p=128), w=["WIN"])
        P.dma("sp", ROPE, rope_d, w=["ROPE"])
        P.op("pool", lambda e: e.memset(UP, 0.0), w=["UP"])
        P.op("pool", lambda e: e.memset(KRT, 0.0), w=["KRT"])
        for b in range(5):
            c0, c1 = BLOCKS[b]
            nb = c1 - c0
            self.norm_mod(b, lambda c, s0, s1: HLB[:, c, s0 - c0:s1 - c0], lslot, 1, 0, ["HLB"])
            def mm(col0, m, bank, rows=128):
                for k in range(DC):
                    P.op("pe", lambda e, k=k: e.matmul(PS[0:m, bank, :nb], lhsT=WIN[:, k, col0:col0 + m], rhs=HLB[:, k, :nb],
                                                      start=(k == 0), stop=(k == DC - 1)), r=["WIN", "HLB"], w=[("PS", bank)])
            for (base, nch, DSTN, G, dim, res) in ((0, 3, QN, QG, 384, "QN"), (384, 2, KVN, KG, 256, "KVN")):
                for i in range(nch):
                    bank = nbank()
                    mm(base + i * 128, 128, bank)
                    P.op("act", lambda e, i=i, bank=bank: e.activation(out=QC[:, i, :nb], in_=PS[:, bank, :nb], func=AF.Copy), r=[("PS", bank)], w=["QC"])
                    P.op("act", lambda e, i=i, bank=bank: e.activation(out=SQ[:, i, :nb], in_=PS[:, bank, :nb], func=AF.Square), r=[("PS", bank)], w=["nSQ"])
                for i in range(nch):
                    P.op("pe", lambda e, i=i: e.matmul(PS[:, 7, :nb], lhsT=self.ONES, rhs=SQ[:, i, :nb], start=(i == 0), stop=(i == nch - 1)),
                         r=["nSQ", "ONES"], w=[("PS", 7)])
                P.op("dve", lambda e: e.tensor_scalar(out=RS[:, :nb], in0=PS[:, 7, :nb], scalar1=1.0 / dim, scalar2=EPS, op0=ALU.mult, op1=ALU.add),
                     r=[("PS", 7)], w=["nRS"])
                P.op("act", lambda e: e.activation(out=RS[:, :nb], in_=RS[:, :nb], func=AF.Sqrt), r=["nRS"], w=["nRS"])
                P.op("dve", lambda e: e.reciprocal(out=RS[:, :nb], in_=RS[:, :nb]), r=["nRS"], w=["nRS"])
                for i in range(nch):
                    P.op("dve", lambda e, i=i: e.scalar_tensor_tensor(out=DSTN[:, i, c0:c1], in0=QC[:, i, :nb], scalar=G[:, i:i + 1], in1=RS[:, :nb],
                                                                     op0=ALU.mult, op1=ALU.mult), r=["QC", "nRS", "PF"], w=[res])
            for g in range(4):
                bank = nbank()
                mm(640 + g * 128, 128, bank)
                for (s0, s1, s) in segs(c0, c1):
                    P.op("act", lambda e, g=g, bank=bank, s0=s0, s1=s1: e.activation(out=UP[:, g, pcol(s0):pcol(s0) + s1 - s0], in_=PS[:, bank, s0 - c0:s1 - c0],
                                                                                    func=AF.Copy), r=[("PS", bank)], w=["UP"])
            bA = nbank()
            mm(1152, 96, bA)
            bB = nbank()
            mm(1248, 96, bB)
            for (s0, s1, s) in segs(c0, c1):
                n = s1 - s0
                if s == 0:
                    T1, T2 = self.nTM[0], self.nTM[1]
                    P.op("dve", lambda e: e.tensor_tensor(out=T1[64:96, :n], in0=PS[64:96, bA, s0 - c0:s1 - c0], in1=ROPE[64:96, 0, s0:s1], op=ALU.mult),
                         r=[("PS", bA), "ROPE"], w=[("nTM", 0)])
                    P.op("dve", lambda e: e.tensor_tensor(out=T2[64:96, :n], in0=PS[64:96, bB, s0 - c0:s1 - c0], in1=ROPE[64:96, 1, s0:s1], op=ALU.mult),
                         r=[("PS", bB), "ROPE"], w=[("nTM", 1)])
                    P.op("pool", lambda e: e.tensor_tensor(out=KRT[64:96, s0:s1], in0=T1[64:96, :n], in1=T2[64:96, :n], op=ALU.add),
                         r=[("nTM", 0), ("nTM", 1)], w=["KRT"])
                else:
                    P.op("act", lambda e: e.activation(out=KRT[64:96, s0:s1], in_=PS[64:96, bA, s0 - c0:s1 - c0], func=AF.Copy), r=[("PS", bA), ("PS", bB)], w=["KRT"])
        for g in range(4):
            for (pc0, m0) in ((PADW, 0), (PADW + NLAT - HALO, 64)):
                P.op("pool", lambda e, g=g, pc0=pc0, m0=m0: e.tensor_tensor(out=UP[:, g, pc0:pc0 + HALO], in0=UP[:, g, pc0:pc0 + HALO],
                                                                           in1=MASK[:, m0:m0 + HALO], op=ALU.mult), r=["UP", "PF"], w=["UP"])
        self.barrier()
        XIa, XOa = XI.ap(), XO.ap()
        for c in range(2):
            P.dma("sp", XIa[c * 128:(c + 1) * 128, :], KVN[:, c, HALO:HALO + NOWN], r=["KVN"], w=["XI"])
        XI2a, XO2a = XI2.ap(), XO2.ap()
        P.dma("sp", XI2a[0:128, :], KRT[:, HALO:HALO + NOWN], r=["KRT"], w=["XI2"])
        P.dma("sp", XI2a[128:256, :], KRT[:, HALO:HALO + NOWN], r=["KRT"], w=["XI2"])
        off = offL
        WO, off = self.carve(off, [128, DC, 1024], BF16)
        WPL, off = self.carve(off, [128, 4, 128], BF16)
        LA, off = self.carve(off, [128, NPAD], F32)
        LB, off = self.carve(off, [128, NPAD], F32)
        YP, off = self.carve(off, [128, 4, 512], BF16)
        BS, off = self.carve(off, [128, 4], F32)
        assert off <= AW
        P.dma("pool", WO, w_out[js].rearrange("(c p) m -> p c m", p=128), w=["WO"])
        P.dma("pool", WPL, w_pool[js].rearrange("g c d -> c g d"), w=["WPL"])
        o, _ = PFM["b_pool"]
        BP = A[:, OFF_PF + o + j * 4:OFF_PF + o + j * 4 + 4]
        o, _ = PFM["s_pool"]
        SP_ = A[:, OFF_PF + o + j * 4:OFF_PF + o + j * 4 + 4]
        P.op("dve", lambda e: e.tensor_tensor(out=BS, in0=BP, in1=SP_, op=ALU.mult), r=["PF"], w=["BS"])
        o, _ = PFM["pcorr_lat"]
        PCL = A[:, OFF_PF + o:OFF_PF + o + 128].rearrange("p (g s) -> p g s", g=4)
        o, _ = PFM["pcorr_ctx"]
        PCC = A[:, OFF_PF + o:OFF_PF + o + 128].rearrange("p (g s) -> p g s", g=4)
        for g in range(4):
            src = UP[:, g, :]
            P.op("pool", lambda e: e.tensor_tensor(out=LA[:, 1:NPAD], in0=src[:, 0:NPAD - 1], in1=src[:, 1:NPAD], op=ALU.add), r=["UP"], w=["LA"])
            cur, oth, cn, on = LA, LB, "LA", "LB"
            lo, hi = 1, NPAD
            for lev in range(1, g + 1):
                d = 1 << (lev - 1)
                lo, hi = lo + d, hi - d
                P.op("pool", lambda e, cur=cur, oth=oth, d=d, lo=lo, hi=hi: e.tensor_tensor(out=oth[:, lo:hi], in0=cur[:, lo - d:hi - d], in1=cur[:, lo + d:hi + d], op=ALU.add),
                     r=[cn], w=[on])
                cur, oth, cn, on = oth, cur, on, cn
            for (pc, tab, t0) in ((PADW + HALO - 8, PCL, 0), (PADW + NLAT - HALO - 8, PCL, 16), (pcol(NLAT), PCC, 0), (pcol(NLAT) + NCTX - 16, PCC, 16)):
                P.op("pool", lambda e, cur=cur, pc=pc, tab=tab, t0=t0, g=g: e.tensor_tensor(out=cur[:, pc:pc + 16], in0=cur[:, pc:pc + 16], in1=tab[:, g, t0:t0 + 16], op=ALU.mult),
                     r=[cn, "PF"], w=[cn])
            w = 2 << g
            P.op("dve", lambda e, cur=cur, g=g, w=w: e.scalar_tensor_tensor(out=UP[:, g, PADW:NPAD - PADW], in0=cur[:, PADW:NPAD - PADW], scalar=1.0 / w,
                                                                       in1=UP[:, g, PADW:NPAD - PADW], op0=ALU.mult, op1=ALU.subtract), r=[cn, "UP"], w=["UP"])
        P.custom("pool", lambda e: e.collective_compute("AllGather", ALU.bypass, replica_groups=[[0, 1, 2, 3], [4, 5, 6, 7]],
                                                        ins=[XI.ap().opt()], outs=[XO.ap().opt()]), (cck, None), r=["XI"], w=["XO"])
        P.wait_all("pool", ["XO"])
        P.custom("pool", lambda e: e.collective_compute("AllGather", ALU.bypass, replica_groups=[[0, 1, 2, 3], [4, 5, 6, 7]],
                                                        ins=[XI2.ap().opt()], outs=[XO2.ap().opt()]), (cck2, None), r=["XI2"], w=["XO2"])
        P.wait_all("pool", ["XO2"])
        for b in range(5):
            c0, c1 = BLOCKS[b]
            nb = c1 - c0
            for g in range(4):
                bank = nbank()
                for (s0, s1, s) in segs(c0, c1):
                    P.op("pe", lambda e, g=g, bank=bank, s0=s0, s1=s1: e.matmul(PS[:, bank, s0 - c0:s1 - c0], lhsT=WPL[:, g, :], rhs=UP[:, g, pcol(s0):pcol(s0) + s1 - s0],
                                                                            start=True, stop=True), r=["WPL", "UP"], w=[("PS", bank)])
                P.op("act", lambda e, g=g, bank=bank: e.activation(out=YP[:, g, :nb], in_=PS[:, bank, :nb], func=AF.Identity, bias=BS[:, g:g + 1], scale=SP_[:, g:g + 1]),
                     r=[("PS", bank), "BS", "PF"], w=["YP"])
            for mc in range(DC):
                bank = nbank()
                for g in range(4):
                    P.op("pe", lambda e, g=g, mc=mc, bank=bank: e.matmul(PS[:, bank, :nb], lhsT=WO[:, 4 + g, mc * 128:(mc + 1) * 128], rhs=YP[:, g, :nb],
                                                                     start=(g == 0), stop=(g == 3)), r=["WO", "YP"], w=[("PS", bank)])
                for (s0, s1, s) in segs(c0, c1):
                    P.op("dve", lambda e, mc=mc, bank=bank, s0=s0, s1=s1, s=s: e.scalar_tensor_tensor(
                        out=XT[:, mc, s0:s1], in0=PS[:, bank, s0 - c0:s1 - c0], scalar=self.mod(lslot, 2, s, mc), in1=XT[:, mc, s0:s1],
                        op0=ALU.mult, op1=ALU.add), r=[("PS", bank), ("XT", b), "MOD"], w=[("XT", b)])
        self.barrier()
        for b, (c0, c1) in enumerate(BLOCKS):
            P.dma("sp", XSP[:, :, c0:c1], XT[:, :, c0:c1], r=[("XT", b)], w=["XSP"])
        P.wait_all("sp", ["XSP"])
        self.barrier()
        offx = OFF_XT
        KVA, offx = self.carve(offx, [128, 2, NKEY], BF16)
        KB, offx = self.carve(offx, [128, NKEY], BF16)
        VB, offx = self.carve(offx, [128, NKT, 128], BF16)
        QB, offx = self.carve(offx, [128, NT], BF16)
        assert offx <= OFF_S
        for rr in range(4):
            for c in range(2):
                P.dma("sp", KVA[:, c, NCTX + rr * NOWN:NCTX + (rr + 1) * NOWN], XOa[rr * 256 + c * 128:rr * 256 + (c + 1) * 128, :], r=["XO"], w=["KVA"])
            P.dma("sp", KB[:, NCTX + rr * NOWN:NCTX + (rr + 1) * NOWN], XO2a[rr * 256:rr * 256 + 128, :], r=["XO2"], w=["KBR", "KB"])
        for c in range(2):
            P.op("pool", lambda e, c=c: e.tensor_copy(out=KVA[:, c, 0:NCTX], in_=KVN[:, c, NLAT:NT]), r=["KVN"], w=["KVA"])
        P.op("pool", lambda e: e.tensor_copy(out=KB[64:96, 0:NCTX], in_=KRT[64:96, NLAT:NT]), r=["KRT"], w=["KBR"])
        off = offL + DC * 1024 // 2
        WUQ, off = self.carve(off, [128, 3, 768], BF16)
        WUQS, off = self.carve(off, [128, 3, 768], BF16)
        WUKV, off = self.carve(off, [128, 2, 1024], BF16)
        PT = []
        for i in range(4):
            t, off = self.carve(off, [128, 512], BF16); PT.append(t)
        OSB, off = self.carve(off, [128, 512], F32)
        RCP, off = self.carve(off, [128, 512], F32)
        T1, off = self.carve(off, [128, 512], F32)
        T2, off = self.carve(off, [128, 512], F32)
        assert off <= AW
        ATT, _ = self.carve(offL - 4 * NPAD // 2, [128, 4, NT], BF16)
        P.op("pool", lambda e: e.memset(ATT, 0.0), w=["ATT"])
        P.dma("pool", WUQ, w_uq[js].rearrange("(c p) m -> p c m", p=128), w=["WUQ"])
        P.dma("pool", WUQS, w_uqs[js].rearrange("(c p) m -> p c m", p=128), w=["WUQS"])
        P.dma("pool", WUKV, w_ukv[js].rearrange("(c p) m -> p c m", p=128), w=["WUKV"])
        qblocks = [(0, 512), (512, 1024), (1024, 1536), (1536, 2048), (2048, NT)] if ctx_out else [(HALO + i * 512, HALO + (i + 1) * 512) for i in range(4)]
        kblocks = [(i * 512, min((i + 1) * 512, NKEY)) for i in range((NKEY + 511) // 512)]
        npt = [0]
        nob = [0]
        nev = [0]
        prot = [0]
        PB = (5, 6, 0, 1, 2, 7)
        def pbank():
            prot[0] += 1
            return PB[prot[0] % len(PB)]
        def evac(dst, src, r, w):
            P.op("dve", lambda e: e.tensor_copy(out=dst, in_=src), r=r, w=w)
        for h in range(8):
            hp = h % 2
            for (k0, k1) in kblocks:
                bank = pbank()
                for c in range(2):
                    P.op("pe", lambda e, c=c: e.matmul(PS[0:64, bank, :k1 - k0], lhsT=WUKV[:, c, h * 128:h * 128 + 64], rhs=KVA[:, c, k0:k1],
                                                      start=(c == 0), stop=(c == 1)), r=["WUKV", "KVA"], w=[("PS", bank)])
                evac(KB[0:64, k0:k1], PS[0:64, bank, :k1 - k0], [("PS", bank)], ["KB"])
            P.op("pool", lambda e: e.memset(VB[:, :, (1 - hp) * 64:(1 - hp) * 64 + 64], 1.0), w=["VB"])
            for kt0 in range(0, NKT, 8):
                nk = min(8, NKT - kt0)
                bank = pbank()
                for jj in range(nk):
                    kt = kt0 + jj
                    for c in range(2):
                        P.op("pe", lambda e, c=c, jj=jj, kt=kt: e.matmul(PS[:, bank, jj * 64:(jj + 1) * 64], lhsT=KVA[:, c, kt * 128:(kt + 1) * 128],
                                                                        rhs=WUKV[:, c, h * 128 + 64:h * 128 + 128], start=(c == 0), stop=(c == 1)),
                             r=["WUKV", "KVA"], w=[("PS", bank)])
                evac(VB[:, kt0:kt0 + nk, hp * 64:hp * 64 + 64], PS[:, bank, 0:nk * 64].rearrange("p (a b) -> p a b", a=nk), [("PS", bank)], ["VB"])
            for (q0, q1) in qblocks:
                nq = q1 - q0
                bA = pbank()
                for c in range(3):
                    P.op("pe", lambda e, c=c: e.matmul(PS[0:96, bA, :nq], lhsT=WUQ[:, c, h * 96:(h + 1) * 96], rhs=QN[:, c, q0:q1], start=(c == 0), stop=(c == 2)),
                         r=["WUQ", "QN"], w=[("PS", bA)])
                bB = pbank()
                for c in range(3):
                    P.op("pe", lambda e, c=c: e.matmul(PS[0:96, bB, :nq], lhsT=WUQS[:, c, h * 96:(h + 1) * 96], rhs=QN[:, c, q0:q1], start=(c == 0), stop=(c == 2)),
                         r=["WUQS", "QN"], w=[("PS", bB)])
                P.op("dve", lambda e: e.tensor_copy(out=QB[0:64, q0:q1], in_=PS[0:64, bA, :nq]), r=[("PS", bA)], w=["QB"])
                for (s0, s1, s) in segs(q0, q1):
                    n = s1 - s0
                    if s == 0:
                        P.op("dve", lambda e: e.tensor_tensor(out=T1[64:96, :n], in0=PS[64:96, bA, s0 - q0:s1 - q0], in1=ROPE[64:96, 0, s0:s1], op=ALU.mult),
                             r=[("PS", bA), "ROPE"], w=["T1"])
                        P.op("dve", lambda e: e.tensor_tensor(out=T2[64:96, :n], in0=PS[64:96, bB, s0 - q0:s1 - q0], in1=ROPE[64:96, 1, s0:s1], op=ALU.mult),
                             r=[("PS", bB), "ROPE"], w=["T2"])
                        P.op("pool", lambda e: e.tensor_tensor(out=QB[64:96, s0:s1], in0=T1[64:96, :n], in1=T2[64:96, :n], op=ALU.add), r=["T1", "T2"], w=["QB"])
                    else:
                        P.op("act", lambda e: e.activation(out=QB[64:96, s0:s1], in_=PS[64:96, bA, s0 - q0:s1 - q0], func=AF.Copy), r=[("PS", bA), ("PS", bB)], w=["QB"])
            jobs = [(q0, min(q1, NLAT), 0, NKT) for (q0, q1) in qblocks]
            if ctx_out:
                jobs.append((NLAT, NT, 0, 2))
            for (q0, q1, kta, ktb) in jobs:
                nq = q1 - q0
                ob = 3 + nob[0] % 2
                nob[0] += 1
                SBK = (0, 1, 2, 7)
                def s_mm(kt):
                    sb = SBK[kt % 4]
                    P.op("pe", lambda e: e.matmul(PS[:, sb, :nq], lhsT=KB[0:96, kt * 128:(kt + 1) * 128], rhs=QB[0:96, q0:q1], start=True, stop=True),
                         r=["KB", "KBR", "QB"], w=[("PS", sb)])
                s_mm(kta)
                if kta + 1 < ktb:
                    s_mm(kta + 1)
                for kt in range(kta, ktb):
                    if kt + 2 < ktb:
                        s_mm(kt + 2)
                    sb = SBK[kt % 4]
                    pi = npt[0] % 4
                    npt[0] += 1
                    P.op("act", lambda e: e.activation(out=PT[pi][:, :nq], in_=PS[:, sb, :nq], func=AF.Exp, scale=MLA_SCALE), r=[("PS", sb)], w=[("PT", pi)])
                    P.op("pe", lambda e: e.matmul(PS[:, ob, :nq], lhsT=VB[:, kt, :], rhs=PT[pi][:, :nq], start=(kt == kta), stop=(kt == ktb - 1)),
                         r=["VB", ("PT", pi)], w=[("PS", ob)])
                nlo, dlo = hp * 64, (1 - hp) * 64
                P.op("dve", lambda e: e.reciprocal(out=RCP[nlo:nlo + 64, :nq], in_=PS[dlo:dlo + 64, ob, :nq]), r=[("PS", ob)], w=["RCP"])
                P.op("dve", lambda e: e.tensor_tensor(out=ATT[nlo:nlo + 64, h // 2, q0:q1], in0=PS[nlo:nlo + 64, ob, :nq], in1=RCP[nlo:nlo + 64, :nq], op=ALU.mult),
                     r=[("PS", ob), "RCP"], w=["ATT"])
        self.barrier()
        for b, (c0, c1) in enumerate(BLOCKS):
            P.dma("sp", XT[:, :, c0:c1], XSP[:, :, c0:c1], r=["XSP"], w=[("XT", b)])
        for b in range(5):
            c0, c1 = BLOCKS[b]
            nb = c1 - c0
            for mc in range(DC):
                bank = nbank(0, 4)
                for k in range(4):
                    P.op("pe", lambda e, k=k, mc=mc: e.matmul(PS[:, bank, :nb], lhsT=WO[:, k, mc * 128:(mc + 1) * 128], rhs=ATT[:, k, c0:c1], start=(k == 0), stop=(k == 3)),
                         r=["WO", "ATT"], w=[("PS", bank)])
                for (s0, s1, s) in segs(c0, c1):
                    P.op("dve", lambda e, mc=mc, s0=s0, s1=s1, s=s: e.scalar_tensor_tensor(
                        out=XT[:, mc, s0:s1], in0=PS[:, bank, s0 - c0:s1 - c0], scalar=self.mod(lslot, 2, s, mc), in1=XT[:, mc, s0:s1],
                        op0=ALU.mult, op1=ALU.add), r=[("PS", bank), ("XT", b), "MOD"], w=[("XT", b)])
        self.barrier()

    def final_store(self):
        P, PS, XT, A = self.P, self.PS, self.XT, self.arena
        out = self.dout("out", [NOWN, 1024])
        off = OFF_S
        off = self.norm_scratch(off)
        XN, off = self.carve(off, [128, DC, 512], F32)
        OT = []
        for i in range(2):
            t, off = self.carve(off, [128, 1024], F32); OT.append(t)
        o, _ = PFM["final_g"]
        FG = A[:, OFF_PF + o:OFF_PF + o + 8]
        SQ, RS = self.nSQ, self.nRS
        nt = 0
        for qb in range(4):
            c0 = HALO + qb * 512
            c1 = c0 + 512
            xr = [("XT", b) for b in range(5)]
            P.op("act", lambda e, c0=c0, c1=c1: e.activation(out=SQ, in_=XT[:, :, c0:c1], func=AF.Square), r=xr, w=["nSQ"])
            for c in range(DC):
                P.op("pe", lambda e, c=c: e.matmul(PS[:, 7, :], lhsT=self.ONES, rhs=SQ[:, c, :], start=(c == 0), stop=(c == DC - 1)),
                     r=["nSQ", "ONES"], w=[("PS", 7)])
            P.op("dve", lambda e: e.tensor_scalar(out=RS, in0=PS[:, 7, :], scalar1=1.0 / D, scalar2=EPS, op0=ALU.mult, op1=ALU.add), r=[("PS", 7)], w=["nRS"])
            P.op("act", lambda e: e.activation(out=RS, in_=RS, func=AF.Sqrt), r=["nRS"], w=["nRS"])
            P.op("dve", lambda e: e.reciprocal(out=RS, in_=RS), r=["nRS"], w=["nRS"])
            for c in range(DC):
                P.op("dve", lambda e, c=c, c0=c0, c1=c1: e.scalar_tensor_tensor(out=XN[:, c, :], in0=XT[:, c, c0:c1], scalar=FG[:, c:c + 1], in1=RS,
                                                                            op0=ALU.mult, op1=ALU.mult), r=xr + ["nRS", "PF"], w=["XN"])
            for tt in range(4):
                i = nt % 2
                nt += 1
                for half in range(2):
                    bank = 2 * i + half
                    for c4 in range(4):
                        c = half * 4 + c4
                        P.op("pe", lambda e, c=c, c4=c4, tt=tt, bank=bank: e.transpose(out=PS[:, bank, c4 * 128:(c4 + 1) * 128],
                                                                                   in_=XN[:, c, tt * 128:(tt + 1) * 128], identity=self.ID),
                             r=["XN", "PF"], w=[("PS", bank)])
                    if half == 0:
                        P.op("act", lambda e, i=i, bank=bank: e.activation(out=OT[i][:, 0:512], in_=PS[:, bank, :], func=AF.Copy), r=[("PS", bank)], w=[("OT", i)])
                    else:
                        P.op("dve", lambda e, i=i, bank=bank: e.tensor_copy(out=OT[i][:, 512:1024], in_=PS[:, bank, :]), r=[("PS", bank)], w=[("OT", i)])
                r0 = qb * 512 + tt * 128
                P.dma("sp", out[r0:r0 + 128, :], OT[i], r=[("OT", i)], w=["OUT"])
        P.wait_all("sp", ["OUT"])
        self.barrier()


def build_program(layers, in_mode, out_mode):
    B = Builder(layers, in_mode, out_mode)
    B.prologue()
    if in_mode == "raw":
        B.load_x_raw()
    else:
        B.load_x_state()
    B.modulation()
    for li, l in enumerate(B.layers):
        if l % 2 == 0:
            B.even_mixer(li, l)
        else:
            B.odd_mixer(li, l)
        B.moe(li, l)
    if out_mode == "final":
        B.final_store()
    else:
        B.store_x_state()
    B.P.run()
    return B


def core_inputs(inp, W, core, layers, B):
    b, r = core // 4, core % 4
    m = {}
    pf, rope = host_pf(inp, core)
    m["pf"] = pf
    names = set(B.dram.keys())
    if "x_in" in names:
        xe = np.zeros((NLAT, 1024), np.float32)
        lo, hi = r * NOWN - HALO, (r + 1) * NOWN + HALO
        slo, shi = max(lo, 0), min(hi, SEQ)
        xe[slo - lo:shi - lo] = inp["x"][b, slo:shi]
        m["x_in"] = xe
        m["ctx_in"] = np.ascontiguousarray(inp["ctx"][b], dtype=np.float32)
    if "rope" in names:
        m["rope"] = rope
    ev = [l // 2 for l in layers if l % 2 == 0]
    od = [l // 2 for l in layers if l % 2 == 1]
    for k in names:
        if k in ("pf", "x_in", "ctx_in", "rope", "st_in", "st_out", "out"):
            continue
        if k.startswith("ev_"):
            m[k] = np.ascontiguousarray(W[k][ev])
        elif k.startswith("od_"):
            m[k] = np.ascontiguousarray(W[k][od])
        else:
            m[k] = np.ascontiguousarray(W[k][list(layers)])
    return m


def state_from_ref(x, xc, core):
    b, r = core // 4, core % 4
    xe = np.zeros((NT, 1024), np.float32)
    lo, hi = r * NOWN - HALO, (r + 1) * NOWN + HALO
    slo, shi = max(lo, 0), min(hi, SEQ)
    xe[slo - lo:shi - lo] = x[b, slo:shi]
    xe[NLAT:] = xc[b]
    return np.ascontiguousarray(xe.reshape(NT, 8, 128).transpose(2, 1, 0)).reshape(128, 8 * NT)


_CACHE = {}


def kernel(**inputs):
    inp = {k: np.asarray(v) for k, v in inputs.items()}
    layers = [0, 1, 2, 3]
    if "prog" not in _CACHE:
        _CACHE["prog"] = build_program(layers, "raw", "final")
    B = _CACHE["prog"]
    W = host_weights(inp)
    maps = [core_inputs(inp, W, core, layers, B) for core in range(8)]
    res = run_bass_kernel_spmd(B.nc, maps, core_ids=list(range(8)))
    out = np.zeros((2, SEQ, D), np.float32)
    for core in range(8):
        b, r = core // 4, core % 4
        out[b, r * NOWN:(r + 1) * NOWN] = np.asarray(res.results[core]["out"], dtype=np.float32)
    return out
```
